# Optimizing a Trainium2 kernel written in Bass

```python
import math
import jax, jax.numpy as jnp
from jax import lax
import numpy as np

D_MODEL = 4096
BATCH = 4
SEQ = 4096
DEPTH = 1

GRID_W = 64
CTX_LEN = 256
MIX_W = D_MODEL
HY_W = MIX_W // 2
ML_W = MIX_W - HY_W
ML_HEADS = 4
ML_DV = ML_W // ML_HEADS
ML_DQK = ML_DV // 2
ML_QK = ML_HEADS * ML_DQK
ML_CHUNK = 64
HY_ORDER = 2
HY_BANDS = 16
HY_EMB = 1 + 2 * HY_BANDS
HY_FH = 64
HY_DECAY_TARGET = 1e-2
HY_FAST_PCT = 0.3
HY_SLOW_PCT = 1.5
HY_MAX_DECAY = math.log(HY_DECAY_TARGET) / HY_FAST_PCT
HY_MIN_DECAY = math.log(HY_DECAY_TARGET) / HY_SLOW_PCT
FFN_DIM = 256 * ((8 * D_MODEL // 3 + 255) // 256)
N_GATES = 2 * 2 * ML_HEADS
P_HY = 3 * HY_W
P_STATE0 = P_HY + ML_QK + ML_W
P_TOTAL = P_STATE0 + ML_QK + ML_W + N_GATES
ALPHA = (2.0 * DEPTH) ** 0.25
BETA = (8.0 * DEPTH) ** -0.25
LN_EPS = 1e-5

kernel_name = "hyena_mlstm_macaron_deepnorm_prefix"


def layer_norm(h, g=None, b=None):
    hf = h.astype(jnp.float32)
    mu = jnp.mean(hf, axis=-1, keepdims=True)
    var = jnp.mean(jnp.square(hf - mu), axis=-1, keepdims=True)
    y = (hf - mu) * lax.rsqrt(var + LN_EPS)
    if g is not None:
        y = y * g + b
    return y.astype(h.dtype)


def ada_mod(cvec, w, b):
    return (jax.nn.silu(cvec) @ w + b).reshape(cvec.shape[0], 9, D_MODEL)


def modulate(h, m, s):
    return h * (1.0 + m[:, None, 3 * s + 1]) + m[:, None, 3 * s]


def gate_of(m, s):
    return m[:, None, 3 * s + 2]


def swiglu(h, wi, wo):
    g, u = jnp.split(h @ wi, 2, axis=-1)
    return (jax.nn.silu(g) * u) @ wo


def conv3(u, w, b):
    n = u.shape[-2]
    up = jnp.pad(u, [(0, 0)] * (u.ndim - 2) + [(1, 1), (0, 0)])
    return up[..., 0:n, :] * w[0] + up[..., 1:n + 1, :] * w[1] + up[..., 2:n + 2, :] * w[2] + b


def latent_conv3(u, w, b):
    B, L, C = u.shape
    rows = L // GRID_W
    return conv3(u.reshape(B, rows, GRID_W, C), w, b).reshape(B, L, C)


def hyena_filters(L, w1, b1, f1, w2, b2, f2, w3):
    t = jnp.linspace(0.0, 1.0, L, dtype=jnp.float32)[:, None]
    w = (2.0 * math.pi / L) * jnp.arange(L, dtype=jnp.float32)[:, None]
    bands = jnp.linspace(1e-4, HY_BANDS - 1, HY_BANDS, dtype=jnp.float32)[None, :]
    z = jnp.concatenate([t, jnp.cos(bands * w), -jnp.sin(bands * w)], axis=-1)
    h = jnp.sin(f1 * (z @ w1 + b1))
    h = jnp.sin(f2 * (h @ w2 + b2))
    h = (h @ w3).astype(jnp.float32).reshape(L, HY_ORDER, 2, HY_W)
    deltas = jnp.abs(jnp.linspace(HY_MIN_DECAY, HY_MAX_DECAY, HY_W, dtype=jnp.float32))
    h = h * jnp.exp(-t * deltas)[:, None, None, :]
    return h / jnp.sum(jnp.abs(h), axis=(0, 2), keepdims=True)


def two_sided(h_f, h_b):
    head = h_f.at[0].add(h_b[0])
    return jnp.concatenate([head, jnp.zeros_like(h_f[:1]), h_b[:0:-1]], axis=0)


def fft_longconv(u, g, bias):
    L = u.shape[1]
    U = jnp.fft.rfft(u.astype(jnp.float32), n=2 * L, axis=1)
    G = jnp.fft.rfft(g.astype(jnp.float32), n=2 * L, axis=0)
    y = jnp.fft.irfft(U * G[None], n=2 * L, axis=1)[:, :L]
    return (y + u.astype(jnp.float32) * bias).astype(u.dtype)


def hyena(u3, filt, hy_bias):
    v, x1, x2 = jnp.split(u3, 3, axis=-1)
    z = x1 * fft_longconv(v, two_sided(filt[:, 0, 0], filt[:, 0, 1]), hy_bias[0])
    return x2 * fft_longconv(z, two_sided(filt[:, 1, 0], filt[:, 1, 1]), hy_bias[1])


def mlstm_state_inputs(p_s, conv_w, conv_b, gate_b, conv_fn):
    B, L, _ = p_s.shape
    k = conv_fn(p_s[..., :ML_QK], conv_w, conv_b).reshape(B, L, ML_HEADS, ML_DQK)
    v = p_s[..., ML_QK:ML_QK + ML_W].reshape(B, L, ML_HEADS, ML_DV)
    g = (p_s[..., ML_QK + ML_W:] + gate_b).astype(jnp.float32).reshape(B, L, 2, 2, ML_HEADS)
    return k, v, g[:, :, :, 0], jax.nn.log_sigmoid(g[:, :, :, 1])


def mlstm_query_inputs(p_q, conv_w, conv_b, conv_fn):
    B, L, _ = p_q.shape
    q = conv_fn(p_q[..., :ML_QK], conv_w, conv_b).reshape(B, L, ML_HEADS, ML_DQK) * (ML_DQK ** -0.5)
    return q, p_q[..., ML_QK:]


def mlstm_scan(k, v, ig, lf, state, q=None):
    B, L = k.shape[:2]
    nc = L // ML_CHUNK

    def chunks(a):
        a = a.astype(jnp.float32)
        return jnp.moveaxis(a.reshape((B, nc, ML_CHUNK) + a.shape[2:]), 1, 0)

    with_out = q is not None
    xs = (chunks(k), chunks(v), chunks(ig), chunks(lf)) + ((chunks(q),) if with_out else ())
    tri = jnp.tril(jnp.ones((ML_CHUNK, ML_CHUNK), dtype=bool))[None, :, :, None]

    def step(carry, inp):
        C, n, m = carry
        kc, vc, igc, lfc = inp[:4]
        b = jnp.cumsum(lfc, axis=1)
        b_end = b[:, -1]
        to_end = b_end[:, None] - b + igc
        m_new = jnp.maximum(b_end + m, jnp.max(to_end, axis=1))
        h = None
        if with_out:
            qc = inp[4]
            dlog = b[:, :, None] - b[:, None] + igc[:, None]
            dlog = jnp.where(tri, dlog, -jnp.inf)
            inter = b + m[:, None]
            m_j = jnp.maximum(inter, jnp.max(dlog, axis=2))
            s = jnp.einsum('bjhd,bshd->bjsh', qc, kc) * jnp.exp(dlog - m_j[:, :, None])
            w_inter = jnp.exp(inter - m_j)
            num = (jnp.einsum('bjsh,bshe->bjhe', s, vc)
                   + w_inter[..., None] * jnp.einsum('bhed,bjhd->bjhe', C, qc))
            den = jnp.sum(s, axis=2) + w_inter * jnp.einsum('bhd,bjhd->bjh', n, qc)
            h = num / jnp.maximum(jnp.abs(den), jnp.exp(-m_j))[..., None]
        w_state = jnp.exp(to_end - m_new[:, None])
        decay = jnp.exp(b_end + m - m_new)
        C_new = decay[..., None, None] * C + jnp.einsum('bshe,bshd->bhed', vc * w_state[..., None], kc)
        n_new = decay[..., None] * n + jnp.einsum('bsh,bshd->bhd', w_state, kc)
        return (C_new, n_new, m_new), h

    state, hs = lax.scan(step, state, xs)
    if not with_out:
        return None, state
    h = jnp.moveaxis(hs, 0, 1).reshape(B, L, ML_HEADS, ML_DV).astype(v.dtype)
    return h, state


def flip_t(*arrs):
    return tuple(jnp.flip(a, axis=1) for a in arrs)


def mlstm_merge(h, o, norm_w):
    B, L = h.shape[:2]
    return layer_norm(h).reshape(B, L, ML_W) * norm_w * jax.nn.sigmoid(o)


def mixer(u_lat, u_ctx, w_in, hy_conv_w, hy_conv_b, filt_params, hy_bias,
          ml_conv_w, ml_conv_b, ml_gate_b, ml_norm_w, w_out, with_ctx_out):
    B = u_ctx.shape[0]
    p_lat = u_lat @ w_in
    pc_state = u_ctx @ w_in[:, P_STATE0:]

    L_lat = u_lat.shape[1]
    hy_lat = hyena(latent_conv3(p_lat[..., :P_HY], hy_conv_w, hy_conv_b),
                   hyena_filters(L_lat, *filt_params), hy_bias)

    kc, vc, igc, lfc = mlstm_state_inputs(pc_state, ml_conv_w[:, ML_QK:], ml_conv_b[ML_QK:], ml_gate_b, conv3)
    kl, vl, igl, lfl = mlstm_state_inputs(p_lat[..., P_STATE0:], ml_conv_w[:, ML_QK:], ml_conv_b[ML_QK:],
                                          ml_gate_b, latent_conv3)
    ql, ol = mlstm_query_inputs(p_lat[..., P_HY:P_STATE0], ml_conv_w[:, :ML_QK], ml_conv_b[:ML_QK], latent_conv3)
    if with_ctx_out:
        pc_rest = u_ctx @ w_in[:, :P_STATE0]
        qc, oc = mlstm_query_inputs(pc_rest[..., P_HY:], ml_conv_w[:, :ML_QK], ml_conv_b[:ML_QK], conv3)
    else:
        qc = None
    zero = (jnp.zeros((B, ML_HEADS, ML_DV, ML_DQK), jnp.float32),
            jnp.zeros((B, ML_HEADS, ML_DQK), jnp.float32),
            jnp.zeros((B, ML_HEADS), jnp.float32))
    hcf, st_f = mlstm_scan(kc, vc, igc[:, :, 0], lfc[:, :, 0], zero, qc)
    hcb, st_b = mlstm_scan(*flip_t(kc, vc, igc[:, :, 1], lfc[:, :, 1]), zero,
                           None if qc is None else jnp.flip(qc, axis=1))
    hlf, _ = mlstm_scan(kl, vl, igl[:, :, 0], lfl[:, :, 0], st_f, ql)
    hlb, _ = mlstm_scan(*flip_t(kl, vl, igl[:, :, 1], lfl[:, :, 1]), st_b, jnp.flip(ql, axis=1))
    ml_lat = mlstm_merge(hlf + jnp.flip(hlb, axis=1), ol, ml_norm_w)
    y_lat = jnp.concatenate([hy_lat, ml_lat], axis=-1) @ w_out

    y_ctx = None
    if with_ctx_out:
        hy_ctx = hyena(conv3(pc_rest[..., :P_HY], hy_conv_w, hy_conv_b),
                       hyena_filters(u_ctx.shape[1], *filt_params), hy_bias)
        ml_ctx = mlstm_merge(hcf + jnp.flip(hcb, axis=1), oc, ml_norm_w)
        y_ctx = jnp.concatenate([hy_ctx, ml_ctx], axis=-1) @ w_out
    return y_lat, y_ctx


def setup_inputs(seed: int = 0) -> dict:
    key = jax.random.key(seed)
    ks = jax.random.split(key, 32)
    D, F, H = D_MODEL, FFN_DIM, ML_HEADS

    def nrm(k, shape, s):
        return s * jax.random.normal(k, shape, jnp.float32)

    i_b = nrm(ks[26], (DEPTH, 2, 1, H), 0.1)
    f_b = jnp.linspace(3.0, 6.0, H, dtype=jnp.float32) + nrm(ks[27], (DEPTH, 2, 1, H), 0.1)
    ml_gate_b = jnp.concatenate([i_b, f_b], axis=2).reshape(DEPTH, N_GATES)
    return {
        "x": nrm(ks[0], (BATCH, SEQ, D), 1.0),
        "c": nrm(ks[1], (BATCH, D), 1.0),
        "ctx": nrm(ks[2], (BATCH, CTX_LEN, D), 1.0),
        "c_ctx": nrm(ks[3], (D,), 1.0),
        "ada_w": nrm(ks[4], (DEPTH, D, 9 * D), D ** -0.5),
        "ada_b": nrm(ks[5], (DEPTH, 9 * D), 0.02),
        "ln_g": 1.0 + nrm(ks[6], (DEPTH, 3, D), 0.02),
        "ln_b": nrm(ks[7], (DEPTH, 3, D), 0.02),
        "ffn1_wi": nrm(ks[8], (DEPTH, D, 2 * F), D ** -0.5),
        "ffn1_wo": nrm(ks[9], (DEPTH, F, D), BETA * F ** -0.5),
        "ffn2_wi": nrm(ks[10], (DEPTH, D, 2 * F), D ** -0.5),
        "ffn2_wo": nrm(ks[11], (DEPTH, F, D), BETA * F ** -0.5),
        "w_in": nrm(ks[12], (DEPTH, D, P_TOTAL), D ** -0.5),
        "hy_conv_w": nrm(ks[13], (DEPTH, 3, P_HY), 3.0 ** -0.5),
        "hy_conv_b": nrm(ks[14], (DEPTH, P_HY), 0.02),
        "hy_filt_w1": nrm(ks[15], (DEPTH, HY_EMB, HY_FH), HY_EMB ** -0.5),
        "hy_filt_b1": nrm(ks[16], (DEPTH, HY_FH), 0.02),
        "hy_filt_f1": 1.0 + nrm(ks[17], (DEPTH, HY_FH), 0.1),
        "hy_filt_w2": nrm(ks[18], (DEPTH, HY_FH, HY_FH), HY_FH ** -0.5),
        "hy_filt_b2": nrm(ks[19], (DEPTH, HY_FH), 0.02),
        "hy_filt_f2": 1.0 + nrm(ks[20], (DEPTH, HY_FH), 0.1),
        "hy_filt_w3": nrm(ks[21], (DEPTH, HY_FH, HY_ORDER * 2 * HY_W), HY_FH ** -0.5),
        "hy_bias": nrm(ks[22], (DEPTH, HY_ORDER, HY_W), 0.5),
        "ml_conv_w": nrm(ks[23], (DEPTH, 3, 2 * ML_QK), 3.0 ** -0.5),
        "ml_conv_b": nrm(ks[24], (DEPTH, 2 * ML_QK), 0.02),
        "ml_gate_b": ml_gate_b,
        "ml_norm_w": 1.0 + nrm(ks[25], (DEPTH, ML_W), 0.02),
        "w_out": nrm(ks[28], (DEPTH, MIX_W, D), BETA * MIX_W ** -0.5),
    }


def reference(x, c, ctx, c_ctx, ada_w, ada_b, ln_g, ln_b, ffn1_wi, ffn1_wo, ffn2_wi, ffn2_wo,
              w_in, hy_conv_w, hy_conv_b, hy_filt_w1, hy_filt_b1, hy_filt_f1, hy_filt_w2,
              hy_filt_b2, hy_filt_f2, hy_filt_w3, hy_bias, ml_conv_w, ml_conv_b, ml_gate_b,
              ml_norm_w, w_out):
    x = layer_norm(x)
    ctx = layer_norm(ctx)
    for l in range(DEPTH):
        keep_ctx = l < DEPTH - 1
        m_lat = ada_mod(c, ada_w[l], ada_b[l])
        m_ctx = ada_mod(c_ctx[None], ada_w[l], ada_b[l])

        x = layer_norm(ALPHA * x + 0.5 * gate_of(m_lat, 0) * swiglu(modulate(x, m_lat, 0), ffn1_wi[l], ffn1_wo[l]),
                       ln_g[l, 0], ln_b[l, 0])
        ctx = layer_norm(ALPHA * ctx + 0.5 * gate_of(m_ctx, 0) * swiglu(modulate(ctx, m_ctx, 0), ffn1_wi[l], ffn1_wo[l]),
                         ln_g[l, 0], ln_b[l, 0])

        filt = (hy_filt_w1[l], hy_filt_b1[l], hy_filt_f1[l], hy_filt_w2[l], hy_filt_b2[l], hy_filt_f2[l], hy_filt_w3[l])
        y_lat, y_ctx = mixer(modulate(x, m_lat, 1), modulate(ctx, m_ctx, 1), w_in[l], hy_conv_w[l], hy_conv_b[l],
                             filt, hy_bias[l], ml_conv_w[l], ml_conv_b[l], ml_gate_b[l], ml_norm_w[l], w_out[l],
                             keep_ctx)
        x = layer_norm(ALPHA * x + gate_of(m_lat, 1) * y_lat, ln_g[l, 1], ln_b[l, 1])
        if keep_ctx:
            ctx = layer_norm(ALPHA * ctx + gate_of(m_ctx, 1) * y_ctx, ln_g[l, 1], ln_b[l, 1])

        x = layer_norm(ALPHA * x + 0.5 * gate_of(m_lat, 2) * swiglu(modulate(x, m_lat, 2), ffn2_wi[l], ffn2_wo[l]),
                       ln_g[l, 2], ln_b[l, 2])
        if keep_ctx:
            ctx = layer_norm(ALPHA * ctx + 0.5 * gate_of(m_ctx, 2) * swiglu(modulate(ctx, m_ctx, 2), ffn2_wi[l], ffn2_wo[l]),
                             ln_g[l, 2], ln_b[l, 2])
    return x
```

```python
import contextlib
import math
import numpy as np
import ml_dtypes
import concourse.bass as bass
import concourse.mybir as mybir
from concourse.bass_utils import run_bass_kernel_spmd

F32 = mybir.dt.float32
BF16 = mybir.dt.bfloat16
AF = mybir.ActivationFunctionType
ALU = mybir.AluOpType
LN_EPS = 1e-5


class Cfg:
    def __init__(s, D=4096, L=4096, CL=256, F=11008, GW=64):
        s.D, s.L, s.CL, s.F, s.GW = D, L, CL, F, GW
        s.DC = D // 128
        s.FC = F // 128
        s.HYW = D // 2
        s.MLW = D - s.HYW
        s.H = 4
        s.DV = s.MLW // 4
        s.DQK = s.DV // 2
        s.MLQK = 4 * s.DQK
        s.NDC = s.DQK // 128
        s.NG = 16
        s.PHY = 3 * s.HYW
        s.PST0 = s.PHY + s.MLQK + s.MLW
        s.PT = s.PST0 + s.MLQK + s.MLW + s.NG
        s.T = L + CL
        s.NT = s.T // 128
        s.NTC = CL // 128
        s.NTL = L // 128
        s.ALPHA = 2.0 ** 0.25
        s.FH = 64
        s.EMB = 33
        s.CB = 512
        s.NCB = s.HYW // s.CB
        s.NF = 2 * L // 128


class _Op:
    __slots__ = ("eng", "fn", "reads", "writes", "dma", "idx", "ms", "sem", "val")

    def __init__(s, eng, fn, reads, writes, dma):
        s.eng, s.fn, s.reads, s.writes, s.dma = eng, fn, tuple(reads), tuple(writes), dma
        s.ms = False
        s.sem = None
        s.val = 0


class Prog:
    def __init__(s, nc, es):
        s.nc, s.es = nc, es
        s.ops = []
        s.engobj = {"pe": nc.tensor, "act": nc.scalar, "dve": nc.vector, "pool": nc.gpsimd, "sp": nc.sync}
        s.engsem, s.engcnt, s.ressem, s.rescnt, s.waited = {}, {}, {}, {}, {}
        s.nsem = 0
        s.nins = 0

    def op(s, eng, fn, reads=(), writes=(), dma=False):
        o = _Op(eng, fn, reads, writes, dma)
        o.idx = len(s.ops)
        s.ops.append(o)
        return o

    def dma(s, eng, out, in_, reads, writes):
        assert len(writes) == 1
        return s.op(eng, lambda e: e.dma_start(out=out, in_=in_), reads, writes, dma=True)

    def _newsem(s):
        s.nsem += 1
        return s.es.enter_context(s.nc.semaphore(f"sm{s.nsem}"))

    def flush(s):
        ops = s.ops
        if not ops:
            return
        lastw, rd_eng, rd_dma = {}, {}, {}
        deps = []
        for o in ops:
            d = set()
            for r in o.reads:
                if r in lastw:
                    d.add(lastw[r])
            for w in o.writes:
                if w in lastw:
                    d.add(lastw[w])
                for i in rd_eng.get(w, {}).values():
                    d.add(i)
                for i in rd_dma.get(w, ()):
                    d.add(i)
            d.discard(o.idx)
            deps.append(d)
            for w in o.writes:
                lastw[w] = o.idx
                rd_eng[w] = {}
                rd_dma[w] = []
            for r in o.reads:
                if r in o.writes:
                    continue
                if o.dma:
                    rd_dma.setdefault(r, []).append(o.idx)
                else:
                    rd_eng.setdefault(r, {})[o.eng] = o.idx
        lastop = {}
        for o in ops:
            if o.dma:
                o.ms = True
            else:
                lastop[o.eng] = o
            for di in deps[o.idx]:
                p = ops[di]
                if p.dma:
                    continue
                if p.eng == "pe" and o.eng == "pe" and not o.dma:
                    continue
                p.ms = True
        for o in lastop.values():
            o.ms = True
        for o in ops:
            if not o.ms:
                continue
            if o.dma:
                r = o.writes[0]
                if r not in s.ressem:
                    s.ressem[r] = s._newsem()
                    s.rescnt[r] = 0
                s.rescnt[r] += 16
                o.sem, o.val = s.ressem[r], s.rescnt[r]
            else:
                e = o.eng
                if e not in s.engsem or s.engcnt[e] >= 30000:
                    s.engsem[e] = s._newsem()
                    s.engcnt[e] = 0
                s.engcnt[e] += 1
                o.sem, o.val = s.engsem[e], s.engcnt[e]
        for o in ops:
            e = s.engobj[o.eng]
            need = {}
            for di in deps[o.idx]:
                p = ops[di]
                if p.sem is None:
                    continue
                k = id(p.sem)
                if k not in need or need[k][1] < p.val:
                    need[k] = (p.sem, p.val)
            for k, (sem, val) in need.items():
                wk = (o.eng, k)
                if s.waited.get(wk, 0) >= val:
                    continue
                s.waited[wk] = val
                e.wait_ge(sem, val)
            ins = o.fn(e)
            s.nins += 1
            if o.ms:
                ins.then_inc(o.sem, 16 if o.dma else 1)
        for en, e in s.engobj.items():
            for e2, sem in s.engsem.items():
                if e2 == en:
                    continue
                wk = (en, id(sem))
                if s.waited.get(wk, 0) < s.engcnt[e2]:
                    s.waited[wk] = s.engcnt[e2]
                    e.wait_ge(sem, s.engcnt[e2])
            for r, sem in s.ressem.items():
                wk = (en, id(sem))
                if s.waited.get(wk, 0) < s.rescnt[r]:
                    s.waited[wk] = s.rescnt[r]
                    e.wait_ge(sem, s.rescnt[r])
        s.ops = []


C_ID, C_ONES, C_MF, C_MB, C_SL0, C_SL2, C_SC0, C_SC2, C_EUP, C_EDN = range(10)
NCST = 10


def host_consts(cfg):
    i = np.arange(128)
    s_, t_ = i[:, None], i[None, :]
    c = np.zeros((128, NCST, 128), np.float32)
    c[:, C_ID] = (s_ == t_)
    c[:, C_ONES] = 1.0
    c[:, C_MF] = (s_ <= t_)
    c[:, C_MB] = (s_ >= t_)
    c[:, C_SL0] = (s_ == t_ - 1) & (t_ % cfg.GW != 0)
    c[:, C_SL2] = (s_ == t_ + 1) & (t_ % cfg.GW != cfg.GW - 1)
    c[:, C_SC0] = (s_ == t_ - 1)
    c[:, C_SC2] = (s_ == t_ + 1)
    c[:, C_EUP] = (s_ == 0) & (t_ == 127)
    c[:, C_EDN] = (s_ == 127) & (t_ == 0)
    L = cfg.L
    t = np.arange(L, dtype=np.float64)[:, None]
    f = np.arange(L, dtype=np.float64)[None, :]
    ang = 2.0 * np.pi * ((t * f) % (2 * L)) / (2 * L)
    fw = np.concatenate([np.cos(ang), np.sin(ang)], axis=1)
    fw[:, L] = np.where(np.arange(L) % 2 == 0, 1.0, -1.0)
    NF, NTL = cfg.NF, cfg.NTL
    fwr = fw.reshape(NTL, 128, NF, 128).transpose(2, 1, 0, 3)
    fwr = np.ascontiguousarray(fwr).astype(ml_dtypes.bfloat16)
    wgt = np.full((2 * L,), 1.0 / L)
    wgt[0] = 0.5 / L
    wgt[L] = 0.5 / L
    iv = (fw * wgt[None, :]).T
    ivr = iv.reshape(NF, 128, NTL, 128).transpose(2, 1, 0, 3)
    ivr = np.ascontiguousarray(ivr).astype(ml_dtypes.bfloat16)
    tl = np.linspace(0.0, 1.0, L, dtype=np.float32)[:, None]
    w = (2.0 * math.pi / L) * np.arange(L, dtype=np.float32)[:, None]
    bands = np.linspace(1e-4, 15.0, 16, dtype=np.float32)[None, :]
    z = np.concatenate([tl, np.cos(bands * w), -np.sin(bands * w)], axis=-1).astype(np.float32)
    zT = np.ascontiguousarray(z.T)
    mx = math.log(1e-2) / 0.3
    mn = math.log(1e-2) / 1.5
    deltas = np.abs(np.linspace(mn, mx, cfg.HYW, dtype=np.float32)).astype(np.float32)[None, :]
    negt = np.ascontiguousarray((-tl[:, 0]).astype(np.float32).reshape(cfg.NTL, 128).T)
    return {"cst": c, "cstb": c.astype(ml_dtypes.bfloat16), "fwr": fwr, "ivr": ivr, "zT": zT,
            "deltas": np.ascontiguousarray(deltas), "negt": np.ascontiguousarray(negt)}


def build(cfg, taps=(), nown=None):
    nc = bass.Bass("TRN2", target_bir_lowering=False)
    D, L, CL, F, T = cfg.D, cfg.L, cfg.CL, cfg.F, cfg.T
    DC, FC, NT, NTC, NTL = cfg.DC, cfg.FC, cfg.NT, cfg.NTC, cfg.NTL
    HYW, MLW, MLQK, DV, DQK, NDC, H = cfg.HYW, cfg.MLW, cfg.MLQK, cfg.DV, cfg.DQK, cfg.NDC, cfg.H
    PHY, PST0, PT = cfg.PHY, cfg.PST0, cfg.PT
    NOWN = NTL if nown is None else nown

    def din(name, shape, dt=F32):
        return nc.dram_tensor(name, list(shape), dt, kind="ExternalInput").ap()

    def dscr(name, shape, dt):
        kind = "ExternalOutput" if name in taps else "Internal"
        return nc.dram_tensor(name, list(shape), dt, kind=kind).ap()

    xin = din("xin", [T, D])
    cvT = din("cvT", [128, DC, 2])
    ada_w = din("ada_w", [D, 9 * D])
    ada_bT = din("ada_bT", [128, 9 * DC])
    ln_g = din("ln_g", [3, D])
    ln_b = din("ln_b", [3, D])
    wi = [din("ffn1_wi", [D, 2 * F]), din("ffn2_wi", [D, 2 * F])]
    wo = [din("ffn1_wo", [F, D]), din("ffn2_wo", [F, D])]
    w_in = din("w_in", [D, PT])
    hy_cw = din("hy_conv_w", [3, PHY])
    hy_cb = din("hy_conv_b", [1, PHY])
    fw1 = din("hy_filt_w1", [cfg.EMB, cfg.FH])
    fb1 = din("hy_filt_b1", [cfg.FH, 1])
    ff1 = din("hy_filt_f1", [cfg.FH, 1])
    fw2 = din("hy_filt_w2", [cfg.FH, cfg.FH])
    fb2 = din("hy_filt_b2", [cfg.FH, 1])
    ff2 = din("hy_filt_f2", [cfg.FH, 1])
    fw3 = din("hy_filt_w3", [cfg.FH, 4 * HYW])
    hy_bias = din("hy_bias", [2, HYW])
    ml_cw = din("ml_conv_w", [3, 2 * MLQK])
    ml_cb = din("ml_conv_b", [1, 2 * MLQK])
    ml_gb = din("ml_gate_b", [1, 16])
    ml_nw = din("ml_norm_w", [1, MLW])
    w_out = din("w_out", [D, D])
    cst_d = din("cst", [128, NCST, 128])
    cstb_d = din("cstb", [128, NCST, 128], BF16)
    fwr_d = din("fwr", [cfg.NF, 128, NTL, 128], BF16)
    ivr_d = din("ivr", [NTL, 128, cfg.NF, 128], BF16)
    zT_d = din("zT", [cfg.EMB, L])
    deltas_d = din("deltas", [1, HYW])
    negt_d = din("negt", [128, NTL])
    out_d = nc.dram_tensor("out", [NOWN * 128, D], F32, kind="ExternalOutput").ap()

    gb_d = dscr("gb_d", [4, 128, D], F32)
    x0_d = dscr("x0_d", [T, D], F32)
    x1_d = dscr("x1_d", [T, D], F32)
    x2_d = dscr("x2_d", [L, D], F32)
    vd_d = dscr("vd_d", [T, D], F32)
    aT_d = dscr("aT_d", [DC, 128, T], BF16)
    hT_d = dscr("hT_d", [FC, 128, T], BF16)
    p_d = dscr("p_d", [T, PT - 16], BF16)
    g_d = dscr("g_d", [T, 16], F32)
    u3_d = dscr("u3_d", [L, PHY], BF16)
    q_d = dscr("q_d", [L, MLQK], BF16)
    k_d = dscr("k_d", [T, MLQK], BF16)
    hw_d = dscr("hw_d", [L, 4 * HYW], F32)
    hsd_d = dscr("hsd_d", [2, 2, L, HYW], BF16)
    gf_d = dscr("gf_d", [cfg.NF, 128, cfg.CB], F32)
    z_d = dscr("z_d", [L, HYW], BF16)
    hf_d = dscr("hf_d", [2, L, MLW], F32)
    mixT_d = dscr("mixT_d", [DC, 128, L], BF16)

    es = contextlib.ExitStack()
    with es:
        P = Prog(nc, es)
        _uid = [0]

        def sb(name, shape, dt=F32, st=es):
            _uid[0] += 1
            return st.enter_context(nc.sbuf_tensor(f"{name}_{_uid[0]}", list(shape), dt))
        ps = [es.enter_context(nc.psum_tensor(f"ps{i}", [128, 512], F32)) for i in range(8)]
        cst = sb("cst_s", [128, NCST, 128])
        cstb = sb("cstb_s", [128, NCST, 128], BF16)
        mT = sb("mT", [128, 2, 9 * DC])
        sc1 = sb("sc1", [128, 2, 3, DC])
        hg = sb("hg", [128, 2, 3, DC])
        epst = sb("epst", [128, 1])
        P.dma("sp", cst[:], cst_d[:, :, :], [], ["cst"])
        P.dma("sp", cstb[:], cstb_d[:, :, :], [], ["cstb"])
        P.op("dve", lambda e: e.memset(epst[:], LN_EPS), [], ["epst"])
        ident = cst[:, C_ID, :]
        ones = cst[:, C_ONES, :]

        def mvec(r, j):
            return mT[:, r, j * DC:(j + 1) * DC]

        with contextlib.ExitStack() as ph:
            sT = sb("sT", [128, DC, 2], F32, ph)
            bT = sb("bT", [128, 9 * DC], F32, ph)
            NR = 3
            ring = [sb(f"adar{i}", [128, DC, 128], F32, ph) for i in range(NR)]
            P.dma("sp", sT[:], cvT[:, :, :], [], ["sT"])
            P.dma("sp", bT[:], ada_bT[:, :], [], ["bT"])
            P.op("act", lambda e: e.activation(out=sT[:], in_=sT[:], func=AF.Silu), ["sT"], ["sT"])
            awv = ada_w.rearrange("(kc p) n -> p kc n", p=128)
            NCC = 9 * DC
            CPB = 128
            LA = NR - 1
            for it in range(NCC + LA):
                if it < NCC:
                    r = it % NR
                    P.dma("sp" if it % 2 == 0 else "act", ring[r][:], awv[:, :, it * 128:(it + 1) * 128], [], [f"adar{r}"])
                if it >= LA:
                    cc = it - LA
                    r = cc % NR
                    bk, lc = cc // CPB, cc % CPB
                    pst = ps[bk][:, 0:2 * CPB].rearrange("p (c r) -> p c r", r=2)
                    for kc in range(DC):
                        P.op("pe", (lambda e, r=r, kc=kc, lc=lc, pst=pst: e.matmul(pst[:, lc, :], lhsT=ring[r][:, kc, :], rhs=sT[:, kc, :], start=(kc == 0), stop=(kc == DC - 1))),
                             [f"adar{r}", "sT"], [f"ps{bk}"])
            for bk in range((NCC + CPB - 1) // CPB):
                n = min(CPB, NCC - bk * CPB)
                pst = ps[bk][:, 0:2 * CPB].rearrange("p (c r) -> p c r", r=2)
                for r in range(2):
                    P.op("dve", (lambda e, bk=bk, n=n, r=r, pst=pst: e.tensor_tensor(out=mT[:, r, bk * CPB:bk * CPB + n], in0=pst[:, 0:n, r], in1=bT[:, bk * CPB:bk * CPB + n], op=ALU.add)),
                         [f"ps{bk}", "bT"], ["mT"])
            for r in range(2):
                for s_ in range(3):
                    P.op("dve", (lambda e, r=r, s_=s_: e.tensor_scalar(out=sc1[:, r, s_, :], in0=mvec(r, 3 * s_ + 1), scalar1=1.0, scalar2=None, op0=ALU.add)), ["mT"], ["sc1"])
                    gsc = 1.0 if s_ == 1 else 0.5
                    P.op("dve", (lambda e, r=r, s_=s_, gsc=gsc: e.tensor_scalar(out=hg[:, r, s_, :], in0=mvec(r, 3 * s_ + 2), scalar1=gsc, scalar2=None, op0=ALU.mult)), ["mT"], ["hg"])
            dg = [sb(f"dg{i}", [128, 128], F32, ph) for i in range(2)]
            gst = [sb(f"gst{i}", [128, 512], F32, ph) for i in range(2)]
            combos = [(0, 0), (1, 0), (0, 1), (0, 2)]
            cnt = 0
            for gi, (r, s_) in enumerate(combos):
                for c4 in range(DC // 4):
                    bk = 4 + (cnt % 2)
                    for ci in range(4):
                        c = c4 * 4 + ci
                        k = (cnt * 4 + ci) % 2
                        P.op("dve", (lambda e, k=k, r=r, s_=s_, c=c: e.tensor_scalar(out=dg[k][:], in0=ident, scalar1=hg[:, r, s_, c:c + 1], scalar2=None, op0=ALU.mult)),
                             ["hg", "cst"], [f"dg{k}"])
                        P.op("pe", (lambda e, k=k, bk=bk, ci=ci: e.matmul(ps[bk][:, ci * 128:(ci + 1) * 128], lhsT=ones, rhs=dg[k][:], start=True, stop=True)),
                             [f"dg{k}", "cst"], [f"ps{bk}"])
                    k2 = cnt % 2
                    P.op("act", (lambda e, k2=k2, bk=bk: e.activation(out=gst[k2][:], in_=ps[bk][:], func=AF.Copy)), [f"ps{bk}"], [f"gst{k2}"])
                    P.dma("sp", gb_d[gi, :, c4 * 512:(c4 + 1) * 512], gst[k2][:], [f"gst{k2}"], ["gb_d"])
                    cnt += 1
            P.flush()

        def ln_phase(tag, tiles, alpha, lng, lnb):
            with contextlib.ExitStack() as ph:
                NB = 2
                vb = [sb(f"vb{i}", [128, D], F32, ph) for i in range(NB)]
                xb = [sb(f"xb{i}", [128, D], F32, ph) for i in range(NB)]
                ust = [sb(f"ust{i}", [128, DC, 128], BF16, ph) for i in range(NB)]
                nch = D // 512
                st = [sb(f"st{i}", [128, nch, 6], F32, ph) for i in range(NB)]
                mv = [sb(f"mv{i}", [128, 4], F32, ph) for i in range(NB)]
                if lng is not None:
                    LNG = sb("LNG", [128, D], F32, ph)
                    LNB = sb("LNB", [128, D], F32, ph)
                    P.dma("sp", LNG[:], lng.partition_broadcast(128), [], ["LNG"])
                    P.dma("sp", LNB[:], lnb.partition_broadcast(128), [], ["LNB"])
                aTv = aT_d.rearrange("c p t -> p c t")
                for i, tl in enumerate(tiles):
                    b = i % NB
                    V, X, ST, MV, US = vb[b], xb[b], st[b], mv[b], ust[b]
                    rv, rx, rst, rmv, rus = f"vb{b}", f"xb{b}", f"st{b}", f"mv{b}", f"ust{b}"
                    P.dma("sp", V[:], tl["src_v"], ["vd_d", "xin"], [rv])
                    if tl.get("src_x") is not None:
                        P.dma("act", X[:], tl["src_x"], ["x0_d", "x1_d"], [rx])
                        P.op("dve", (lambda e, V=V, X=X: e.scalar_tensor_tensor(out=V[:], in0=X[:], scalar=alpha, in1=V[:], op0=ALU.mult, op1=ALU.add)), [rx, rv], [rv])
                    for c in range(nch):
                        P.op("dve", (lambda e, V=V, ST=ST, c=c: e.bn_stats(out=ST[:, c, :], in_=V[:, c * 512:(c + 1) * 512])), [rv], [rst])
                    P.op("dve", (lambda e, ST=ST, MV=MV: e.bn_aggr(out=MV[:, 0:2], in_=ST[:].rearrange("p c s -> p (c s)"))), [rst], [rmv])
                    P.op("act", (lambda e, MV=MV: e.activation(out=MV[:, 2:3], in_=MV[:, 1:2], func=AF.Sqrt, bias=epst[:, 0:1], scale=1.0)), [rmv, "epst"], [rmv])
                    P.op("dve", (lambda e, MV=MV: e.reciprocal(out=MV[:, 2:3], in_=MV[:, 2:3])), [rmv], [rmv])
                    P.op("dve", (lambda e, MV=MV: e.scalar_tensor_tensor(out=MV[:, 3:4], in0=MV[:, 0:1], scalar=-1.0, in1=MV[:, 2:3], op0=ALU.mult, op1=ALU.mult)), [rmv], [rmv])
                    P.op("act", (lambda e, V=V, MV=MV: e.activation(out=V[:], in_=V[:], func=AF.Identity, bias=MV[:, 3:4], scale=MV[:, 2:3])), [rv, rmv], [rv])
                    if lng is not None:
                        P.op("pool", (lambda e, V=V: e.tensor_tensor(out=V[:], in0=V[:], in1=LNG[:], op=ALU.mult)), [rv, "LNG"], [rv])
                        P.op("pool", (lambda e, V=V: e.tensor_tensor(out=V[:], in0=V[:], in1=LNB[:], op=ALU.add)), [rv, "LNB"], [rv])
                    if tl.get("dst_x") is not None:
                        P.dma("sp", tl["dst_x"], V[:], [rv], ["dstx"])
                    if tl.get("dstT") is not None:
                        r, s_ = tl["r"], tl["s"]
                        for c in range(DC):
                            bk = (c // 4) % 2
                            P.op("pe", (lambda e, V=V, c=c, bk=bk: e.transpose(out=ps[bk][:, (c % 4) * 128:(c % 4 + 1) * 128], in_=V[:, c * 128:(c + 1) * 128], identity=ident)),
                                 [rv, "cst"], [f"ps{bk}"])
                            P.op("act", (lambda e, US=US, c=c, bk=bk, r=r, s_=s_: e.activation(out=US[:, c, :], in_=ps[bk][:, (c % 4) * 128:(c % 4 + 1) * 128], func=AF.Identity,
                                                                                    bias=mvec(r, 3 * s_)[:, c:c + 1], scale=sc1[:, r, s_, c:c + 1])),
                                 [f"ps{bk}", "mT", "sc1"], [rus])
                        t0 = tl["dstT"]
                        P.dma("sp", aTv[:, :, t0:t0 + 128], US[:], [rus], ["aT_d"])
                P.flush()

        def gemm_a(Wi, tok0, Tsg):
            with contextlib.ExitStack() as ph:
                xT = sb("xT", [128, DC, Tsg], BF16, ph)
                NR = 3
                wg = [sb(f"wg{i}", [128, DC, 128], BF16, ph) for i in range(NR)]
                wu = [sb(f"wu{i}", [128, DC, 128], BF16, ph) for i in range(NR)]
                hb = [sb(f"hb{i}", [128, Tsg], BF16, ph) for i in range(2)]
                sg = [sb(f"sg{i}", [128, 512], F32, ph) for i in range(2)]
                aTv = aT_d.rearrange("c p t -> p c t")
                q4 = max(1, DC // 4)
                for a in range(0, DC, q4):
                    P.dma("sp", xT[:, a:a + q4, :], aTv[:, a:a + q4, tok0:tok0 + Tsg], ["aT_d"], ["xT"])
                groups = [(t0, min(512, Tsg - t0)) for t0 in range(0, Tsg, 512)]
                wv = Wi.rearrange("(kc p) n -> p kc n", p=128)
                hTv = hT_d
                LA = NR - 1
                gcnt = 0
                for it in range(FC + LA):
                    if it < FC:
                        r = it % NR
                        P.dma("pool", wg[r][:], wv[:, :, it * 128:(it + 1) * 128], [], [f"wg{r}"])
                        P.dma("pool", wu[r][:], wv[:, :, F + it * 128:F + (it + 1) * 128], [], [f"wu{r}"])
                    if it >= LA:
                        j = it - LA
                        r = j % NR
                        hbj = hb[j % 2]
                        for (t0, n) in groups:
                            pg, pu = (gcnt % 4) * 2, (gcnt % 4) * 2 + 1
                            k = gcnt % 2
                            gcnt += 1
                            for kc in range(DC):
                                P.op("pe", (lambda e, r=r, kc=kc, pg=pg, t0=t0, n=n: e.matmul(ps[pg][:, 0:n], lhsT=wg[r][:, kc, :], rhs=xT[:, kc, t0:t0 + n], start=(kc == 0), stop=(kc == DC - 1))),
                                     [f"wg{r}", "xT"], [f"ps{pg}"])
                            for kc in range(DC):
                                P.op("pe", (lambda e, r=r, kc=kc, pu=pu, t0=t0, n=n: e.matmul(ps[pu][:, 0:n], lhsT=wu[r][:, kc, :], rhs=xT[:, kc, t0:t0 + n], start=(kc == 0), stop=(kc == DC - 1))),
                                     [f"wu{r}", "xT"], [f"ps{pu}"])
                            P.op("act", (lambda e, k=k, pg=pg, n=n: e.activation(out=sg[k][:, 0:n], in_=ps[pg][:, 0:n], func=AF.Silu)), [f"ps{pg}"], [f"sg{k}"])
                            P.op("dve", (lambda e, k=k, pu=pu, n=n, t0=t0, hbj=hbj: e.tensor_tensor(out=hbj[:, t0:t0 + n], in0=sg[k][:, 0:n], in1=ps[pu][:, 0:n], op=ALU.mult)),
                                 [f"sg{k}", f"ps{pu}"], [f"hb{j % 2}"])
                        P.dma("sp", hTv[j, :, tok0:tok0 + Tsg], hbj[:], [f"hb{j % 2}"], ["hT_d"])
                P.flush()

        def gemm_tok(tag, aTd, KC, Wd, Ncols, tgroups, epi, extra_setup=None, col_filter=None):
            with contextlib.ExitStack() as ph:
                KP = 8
                NR = 4
                aT = [sb(f"aT{i}", [128, KC, 512], BF16, ph) for i in range(1)]
                wr = [sb(f"wr{i}", [128, KP, 512], BF16, ph) for i in range(NR)]
                state = extra_setup(ph) if extra_setup is not None else None
                aTv = aTd.rearrange("c p t -> p c t")
                wv = Wd.rearrange("(j p) n -> p j n", p=128)
                ncolt = (Ncols + 511) // 512
                pieces = [(j0, min(KP, KC - j0)) for j0 in range(0, KC, KP)]
                par = 0
                for g in tgroups:
                    ntok = 128 * len(g)
                    tok0 = g[0] * 128
                    assert all(g[i] == g[0] + i for i in range(len(g)))
                    q4 = max(1, KC // 4)
                    for a in range(0, KC, q4):
                        a1 = min(KC, a + q4)
                        P.dma("sp", aT[0][:, a:a1, 0:ntok], aTv[:, a:a1, tok0:tok0 + ntok], [], ["aT0"])
                    work = []
                    for n in range(ncolt):
                        if col_filter is not None and not col_filter(g, n):
                            continue
                        for (j0, kp) in pieces:
                            work.append((n, j0, kp))
                    LA = NR - 1
                    for it in range(len(work) + LA):
                        if it < len(work):
                            n, j0, kp = work[it]
                            n0 = n * 512
                            nw = min(512, Ncols - n0)
                            r = it % NR
                            P.dma("pool", wr[r][:, 0:kp, 0:nw], wv[:, j0:j0 + kp, n0:n0 + nw], [], [f"wr{r}"])
                        if it >= LA:
                            n, j0, kp = work[it - LA]
                            n0 = n * 512
                            nw = min(512, Ncols - n0)
                            r = (it - LA) % NR
                            if j0 == 0:
                                par ^= 1
                            for jj in range(kp):
                                j = j0 + jj
                                for ti in range(len(g)):
                                    bk = par * 4 + ti
                                    P.op("pe", (lambda e, r=r, jj=jj, j=j, ti=ti, bk=bk, nw=nw: e.matmul(ps[bk][:, 0:nw], lhsT=aT[0][:, j, ti * 128:(ti + 1) * 128], rhs=wr[r][:, jj, 0:nw], start=(j == 0), stop=(j == KC - 1))),
                                         [f"wr{r}", "aT0"], [f"ps{bk}"])
                            if j0 + kp == KC:
                                for ti in range(len(g)):
                                    epi(state, g[ti], n0, nw, par * 4 + ti)
                P.flush()

        def gated_setup(gidx_of_tile):
            def setup(ph):
                GB = {}
                for gi in sorted(set(gidx_of_tile.values())):
                    GB[gi] = sb(f"GB{gi}", [128, D], F32, ph)
                    P.dma("sp", GB[gi][:], gb_d[gi, :, :], ["gb_d"], [f"GB{gi}"])
                tmp = [sb(f"etmp{i}", [128, 512], F32, ph) for i in range(4)]
                return {"GB": GB, "tmp": tmp, "cnt": [0], "gmap": gidx_of_tile}
            return setup

        def gated_epi(dst_rows):
            def epi(stt, gt, n0, nw, bk):
                k = stt["cnt"][0] % 4
                stt["cnt"][0] += 1
                gi = stt["gmap"][gt]
                G = stt["GB"][gi]
                tmp = stt["tmp"][k]
                P.op("dve", (lambda e: e.tensor_tensor(out=tmp[:, 0:nw], in0=ps[bk][:, 0:nw], in1=G[:, n0:n0 + nw], op=ALU.mult)), [f"ps{bk}", f"GB{gi}"], [f"etmp{k}"])
                r0 = dst_rows(gt)
                P.dma("sp", vd_d[r0:r0 + 128, n0:n0 + nw], tmp[:, 0:nw], [f"etmp{k}"], ["vd_d"])
            return epi

        tiles = []
        for gt in range(NT):
            r = 1 if gt < NTC else 0
            tiles.append(dict(src_v=xin[gt * 128:(gt + 1) * 128, :], dst_x=x0_d[gt * 128:(gt + 1) * 128, :], dstT=gt * 128, r=r, s=0))
        ln_phase("ln0", tiles, 1.0, None, None)

        nsg = 2 if NT > 17 else 1
        per = (NT + nsg - 1) // nsg
        for sgi in range(nsg):
            t0 = sgi * per
            t1 = min(NT, t0 + per)
            gemm_a(wi[0], t0 * 128, (t1 - t0) * 128)
        allg = [list(range(a, min(a + 4, NT))) for a in range(0, NT, 4)]
        gmap = {gt: (1 if gt < NTC else 0) for gt in range(NT)}
        gemm_tok("ffn1b", hT_d, FC, wo[0], D, allg, gated_epi(lambda gt: gt * 128), gated_setup(gmap))
        tiles = []
        for gt in range(NT):
            r = 1 if gt < NTC else 0
            tiles.append(dict(src_v=vd_d[gt * 128:(gt + 1) * 128, :], src_x=x0_d[gt * 128:(gt + 1) * 128, :], dst_x=x1_d[gt * 128:(gt + 1) * 128, :], dstT=gt * 128, r=r, s=1))
        ln_phase("ln1", tiles, cfg.ALPHA, ln_g[0, :], ln_b[0, :])

        if "stop_ffn1" in taps:
            P.flush()
            return nc

        def win_setup(ph):
            return {"tmp": [sb(f"ptmp{i}", [128, 512], BF16, ph) for i in range(4)],
                    "gt": [sb(f"gtmp{i}", [128, 16], F32, ph) for i in range(2)], "cnt": [0]}

        def win_epi(stt, gt, n0, nw, bk):
            c = stt["cnt"][0]
            stt["cnt"][0] += 1
            if n0 >= PT - 16:
                k = c % 2
                tmp = stt["gt"][k]
                P.op("dve", (lambda e: e.tensor_copy(out=tmp[:, 0:nw], in_=ps[bk][:, 0:nw])), [f"ps{bk}"], [f"gtmp{k}"])
                P.dma("sp", g_d[gt * 128:(gt + 1) * 128, 0:nw], tmp[:, 0:nw], [f"gtmp{k}"], ["g_d"])
            else:
                k = c % 4
                tmp = stt["tmp"][k]
                if c % 2 == 0:
                    P.op("act", (lambda e: e.activation(out=tmp[:, 0:nw], in_=ps[bk][:, 0:nw], func=AF.Copy)), [f"ps{bk}"], [f"ptmp{k}"])
                else:
                    P.op("dve", (lambda e: e.tensor_copy(out=tmp[:, 0:nw], in_=ps[bk][:, 0:nw])), [f"ps{bk}"], [f"ptmp{k}"])
                P.dma("sp", p_d[gt * 128:(gt + 1) * 128, n0:n0 + nw], tmp[:, 0:nw], [f"ptmp{k}"], ["p_d"])

        gemm_tok("win", aT_d, DC, w_in, PT, allg, win_epi, win_setup)

        def conv_cols(p_col0, ncols, cw, cbias, dst, dst_col0, scale, with_ctx):
            with contextlib.ExitStack() as ph:
                NB = 2
                Wc = [sb(f"Wc{i}", [128, 3, 512], F32, ph) for i in range(NB)]
                Bc = [sb(f"Bc{i}", [128, 512], F32, ph) for i in range(NB)]
                pt = [sb(f"pt{i}", [128, 512], BF16, ph) for i in range(4)]
                pw = [[sb(f"pw{i}_{k}", [128, 512], BF16, ph) for k in range(3)] for i in range(4)]
                ot = [sb(f"ot{i}", [128, 512], BF16, ph) for i in range(2)]
                cnt = 0
                for cb in range(ncols // 512):
                    b = cb % NB
                    c0 = cb * 512
                    for k in range(3):
                        P.dma("sp", Wc[b][:, k, :], cw[k, c0:c0 + 512].partition_broadcast(128), [], [f"Wc{b}"])
                    P.dma("sp", Bc[b][:], cbias[0, c0:c0 + 512].partition_broadcast(128), [], [f"Bc{b}"])
                    if scale != 1.0:
                        P.op("dve", (lambda e, b=b: e.tensor_scalar(out=Bc[b][:], in0=Bc[b][:], scalar1=scale, scalar2=None, op0=ALU.mult)), [f"Bc{b}"], [f"Bc{b}"])

                    def prods(gt, slot):
                        P.dma("act", pt[slot][:], p_d[gt * 128:(gt + 1) * 128, p_col0 + c0:p_col0 + c0 + 512], ["p_d"], [f"pt{slot}"])
                        for k in range(3):
                            P.op("pool" if k < 2 else "dve", (lambda e, k=k, slot=slot, b=b: e.tensor_tensor(out=pw[slot][k][:], in0=pt[slot][:], in1=Wc[b][:, k, :], op=ALU.mult)),
                                 [f"pt{slot}", f"Wc{b}"], [f"pw{slot}_{k}"])

                    def finish(gt, bk, terms):
                        nonlocal cnt
                        for i, (cidx, slot, k) in enumerate(terms):
                            P.op("pe", (lambda e, cidx=cidx, slot=slot, k=k, i=i, bk=bk, n=len(terms): e.matmul(ps[bk][:], lhsT=cstb[:, cidx, :], rhs=pw[slot][k][:], start=(i == 0), stop=(i == n - 1))),
                                 ["cstb", f"pw{slot}_{k}"], [f"ps{bk}"])
                        o = cnt % 2
                        cnt += 1
                        P.op("dve", (lambda e, o=o, bk=bk, b=b: e.scalar_tensor_tensor(out=ot[o][:], in0=ps[bk][:], scalar=scale, in1=Bc[b][:], op0=ALU.mult, op1=ALU.add)),
                             [f"ps{bk}", f"Bc{b}"], [f"ot{o}"])
                        r0 = gt * 128 if with_ctx else (gt - NTC) * 128
                        P.dma("sp", dst[r0:r0 + 128, dst_col0 + c0:dst_col0 + c0 + 512], ot[o][:], [f"ot{o}"], ["convdst"])

                    if with_ctx:
                        assert NTC == 2
                        prods(0, 0)
                        prods(1, 1)
                        finish(0, 0, [(C_SC0, 0, 0), (C_ID, 0, 1), (C_SC2, 0, 2), (C_EUP, 1, 2)])
                        finish(1, 1, [(C_SC0, 1, 0), (C_ID, 1, 1), (C_SC2, 1, 2), (C_EDN, 0, 0)])
                    for gt in range(NTC, NT):
                        slot = gt % 4
                        prods(gt, slot)
                        finish(gt, gt % 4, [(C_SL0, slot, 0), (C_ID, slot, 1), (C_SL2, slot, 2)])
                P.flush()

        conv_cols(0, PHY, hy_cw, hy_cb, u3_d, 0, 1.0, False)
        conv_cols(PHY, MLQK, ml_cw[:, 0:MLQK], ml_cb[:, 0:MLQK], q_d, 0, float(DQK) ** -0.5, False)
        conv_cols(PST0, MLQK, ml_cw[:, MLQK:2 * MLQK], ml_cb[:, MLQK:2 * MLQK], k_d, 0, 1.0, True)

        if "stop_conv" in taps:
            P.flush()
            return nc

        CB, NCB, NF, NH = cfg.CB, cfg.NCB, cfg.NF, cfg.NF // 2
        FH = cfg.FH
        PI = math.pi
        gfa_d = dscr("gfa_d", [NCB, 2, NF, 128, CB], F32)

        with contextlib.ExitStack() as ph:
            zT = sb("zT", [cfg.EMB, L], F32, ph)
            w1 = sb("w1", [cfg.EMB, FH], F32, ph)
            w2 = sb("w2", [FH, FH], F32, ph)
            w3 = sb("w3", [FH, 4 * HYW], F32, ph)
            fv = sb("fv", [FH, 6], F32, ph)
            h1T = sb("h1T", [FH, L], F32, ph)
            h2T = sb("h2T", [FH, L], F32, ph)
            negt = sb("negt", [128, NTL], F32, ph)
            dlt = sb("dlt", [128, HYW], F32, ph)
            P.dma("sp", zT[:], zT_d[:, :], [], ["zT"])
            P.dma("sp", w1[:], fw1[:, :], [], ["w1"])
            P.dma("sp", w2[:], fw2[:, :], [], ["w2"])
            P.dma("act", w3[:], fw3[:, :], [], ["w3"])
            for i, a in enumerate([fb1, ff1, fb2, ff2]):
                P.dma("sp", fv[:, i:i + 1], a[:, :], [], ["fv"])
            P.dma("sp", negt[:], negt_d[:, :], [], ["negt"])
            P.dma("sp", dlt[:], deltas_d[0, :].partition_broadcast(128), [], ["dlt"])
            P.op("dve", lambda e: e.tensor_tensor(out=fv[:, 4:5], in0=fv[:, 0:1], in1=fv[:, 1:2], op=ALU.mult), ["fv"], ["fv"])
            P.op("dve", lambda e: e.tensor_tensor(out=fv[:, 5:6], in0=fv[:, 2:3], in1=fv[:, 3:4], op=ALU.mult), ["fv"], ["fv"])
            atmp = [sb(f"atmp{i}", [FH, 512], F32, ph) for i in range(2)]
            s4t = [sb(f"s4t{i}", [FH, 512], F32, ph) for i in range(2)]
            s8t = [sb(f"s8t{i}", [FH, 512], F32, ph) for i in range(2)]

            def sin_layer(wt, K, src, dst, fcol, fbcol):
                dres = "h1T" if dst is h1T else "h2T"
                for bi, t0 in enumerate(range(0, L, 512)):
                    k = bi % 2
                    bk = bi % 2
                    A, S4, S8 = atmp[k], s4t[k], s8t[k]
                    ra, r4, r8 = f"atmp{k}", f"s4t{k}", f"s8t{k}"
                    P.op("pe", (lambda e, t0=t0, bk=bk: e.matmul(ps[bk][0:FH, :], lhsT=wt[0:K, :], rhs=src[0:K, t0:t0 + 512], start=True, stop=True)), ["w1", "w2", "zT", "h1T"], [f"ps{bk}"])
                    P.op("act", (lambda e, A=A, bk=bk: e.activation(out=A[:], in_=ps[bk][0:FH, :], func=AF.Identity, bias=fv[:, fbcol:fbcol + 1], scale=fv[:, fcol:fcol + 1])), [f"ps{bk}", "fv"], [ra])
                    P.op("act", (lambda e, A=A, S4=S4: e.activation(out=S4[:], in_=A[:], func=AF.Sin, scale=0.25)), [ra], [r4])
                    P.op("act", (lambda e, A=A, S8=S8: e.activation(out=S8[:], in_=A[:], func=AF.Sin, scale=0.125)), [ra], [r8])
                    P.op("dve", (lambda e, S8=S8: e.tensor_tensor(out=S8[:], in0=S8[:], in1=S8[:], op=ALU.mult)), [r8], [r8])
                    P.op("dve", (lambda e, S8=S8: e.tensor_scalar(out=S8[:], in0=S8[:], scalar1=-2.0, scalar2=1.0, op0=ALU.mult, op1=ALU.add)), [r8], [r8])
                    P.op("dve", (lambda e, S8=S8, S4=S4: e.scalar_tensor_tensor(out=S8[:], in0=S4[:], scalar=2.0, in1=S8[:], op0=ALU.mult, op1=ALU.mult)), [r4, r8], [r8])
                    P.op("dve", (lambda e, S4=S4: e.tensor_tensor(out=S4[:], in0=S4[:], in1=S4[:], op=ALU.mult)), [r4], [r4])
                    P.op("dve", (lambda e, S4=S4: e.tensor_scalar(out=S4[:], in0=S4[:], scalar1=-2.0, scalar2=1.0, op0=ALU.mult, op1=ALU.add)), [r4], [r4])
                    P.op("dve", (lambda e, S4=S4, S8=S8, t0=t0: e.scalar_tensor_tensor(out=dst[:, t0:t0 + 512], in0=S8[:], scalar=2.0, in1=S4[:], op0=ALU.mult, op1=ALU.mult)), [r4, r8], [dres])

            sin_layer(w1, cfg.EMB, zT, h1T, 1, 4)
            sin_layer(w2, FH, h1T, h2T, 3, 5)
            win = sb("win", [128, NTL, 512], F32, ph)
            hwt = [sb(f"hwt{i}", [128, 512], F32, ph) for i in range(4)]
            hab = [sb(f"hab{i}", [128, 512], F32, ph) for i in range(2)]
            rn = sb("rn", [128, 512], F32, ph)
            lf = [sb(f"lf{i}", [128, 2, 512], F32, ph) for i in range(2)]
            hso = [sb(f"hso{i}", [128, 2, 512], BF16, ph) for i in range(2)]
            cnt = 0
            for cbk in range(HYW // 512):
                c0 = cbk * 512
                for tt in range(NTL):
                    P.op("act", (lambda e, tt=tt, c0=c0: e.activation(out=win[:, tt, :], in_=dlt[:, c0:c0 + 512], func=AF.Exp, scale=negt[:, tt:tt + 1])), ["negt", "dlt"], ["win"])
                for o in range(2):
                    for tt in range(NTL):
                        for dr in range(2):
                            col = o * 2 * HYW + dr * HYW + c0
                            bk = 2 + cnt % 2
                            k = cnt % 4
                            ka = cnt % 2
                            cnt += 1
                            P.op("pe", (lambda e, tt=tt, col=col, bk=bk: e.matmul(ps[bk][:], lhsT=h2T[:, tt * 128:(tt + 1) * 128], rhs=w3[:, col:col + 512], start=True, stop=True)), ["h2T", "w3"], [f"ps{bk}"])
                            P.op("dve", (lambda e, tt=tt, bk=bk, k=k: e.tensor_tensor(out=hwt[k][:], in0=ps[bk][:], in1=win[:, tt, :], op=ALU.mult)), [f"ps{bk}", "win"], [f"hwt{k}"])
                            P.dma("sp", hw_d[tt * 128:(tt + 1) * 128, col:col + 512], hwt[k][:], [f"hwt{k}"], ["hw_d"])
                            P.op("act", (lambda e, k=k, ka=ka: e.activation(out=hab[ka][:], in_=hwt[k][:], func=AF.Abs)), [f"hwt{k}"], [f"hab{ka}"])
                            first = (tt == 0 and dr == 0)
                            last = (tt == NTL - 1 and dr == 1)
                            P.op("pe", (lambda e, ka=ka, first=first, last=last: e.matmul(ps[4][:], lhsT=ones, rhs=hab[ka][:], start=first, stop=last)), [f"hab{ka}", "cst"], ["ps4"])
                    P.op("dve", lambda e: e.reciprocal(out=rn[:], in_=ps[4][:]), ["ps4"], ["rn"])
                    for tt in range(NTL):
                        k = tt % 2
                        for dr in range(2):
                            col = o * 2 * HYW + dr * HYW + c0
                            P.dma("act", lf[k][:, dr, :], hw_d[tt * 128:(tt + 1) * 128, col:col + 512], ["hw_d"], [f"lf{k}"])
                        P.op("pool", (lambda e, k=k: e.tensor_tensor(out=hab[0][:], in0=lf[k][:, 0, :], in1=lf[k][:, 1, :], op=ALU.add)), [f"lf{k}"], ["hab0"])
                        P.op("pool", (lambda e, k=k: e.tensor_tensor(out=hab[1][:], in0=lf[k][:, 0, :], in1=lf[k][:, 1, :], op=ALU.subtract)), [f"lf{k}"], ["hab1"])
                        P.op("dve", (lambda e, k=k: e.tensor_tensor(out=hso[k][:, 0, :], in0=hab[0][:], in1=rn[:], op=ALU.mult)), ["hab0", "rn"], [f"hso{k}"])
                        P.op("dve", (lambda e, k=k: e.tensor_tensor(out=hso[k][:, 1, :], in0=hab[1][:], in1=rn[:], op=ALU.mult)), ["hab1", "rn"], [f"hso{k}"])
                        for sd in range(2):
                            P.dma("sp", hsd_d[o, sd, tt * 128:(tt + 1) * 128, c0:c0 + 512], hso[k][:, sd, :], [f"hso{k}"], ["hsd_d"])
            P.flush()

        if "stop_filt" in taps:
            return nc

        def load_tok_tiles(dst, src2d, col0, res):
            v = src2d.rearrange("(tt p) c -> p tt c", p=128)
            q4 = max(1, NTL // 4)
            for a in range(0, NTL, q4):
                P.dma("act", dst[:, a:a + q4, :], v[:, a:a + q4, col0:col0 + CB], ["hsd_d", "u3_d", "z_d"], [res])

        NRF = 4
        for cbk in range(NCB):
            c0 = cbk * CB
            for o in range(2):
                with contextlib.ExitStack() as ph:
                    Hs = sb("Hs", [128, NTL, CB], BF16, ph)
                    Hd = sb("Hd", [128, NTL, CB], BF16, ph)
                    fwb = [sb(f"fwb{i}", [128, NTL, 128], BF16, ph) for i in range(NRF)]
                    go = [sb(f"go{i}", [128, CB], F32, ph) for i in range(2)]
                    load_tok_tiles(Hs, hsd_d[o, 0], c0, "Hs")
                    load_tok_tiles(Hd, hsd_d[o, 1], c0, "Hd")
                    LA = NRF - 1
                    for it in range(NF + LA):
                        if it < NF:
                            r = it % NRF
                            P.dma("sp", fwb[r][:], fwr_d[it, :, :, :], [], [f"fwb{r}"])
                        if it >= LA:
                            fc = it - LA
                            r = fc % NRF
                            bk = fc % 2
                            src, rs = (Hs, "Hs") if fc < NH else (Hd, "Hd")
                            for tt in range(NTL):
                                P.op("pe", (lambda e, r=r, tt=tt, bk=bk, src=src: e.matmul(ps[bk][:, 0:CB], lhsT=fwb[r][:, tt, :], rhs=src[:, tt, :], start=(tt == 0), stop=(tt == NTL - 1))), [f"fwb{r}", rs], [f"ps{bk}"])
                            if fc == NH:
                                for tt in range(NTL):
                                    P.op("pe", (lambda e, r=r, tt=tt: e.matmul(ps[2][0:1, 0:CB], lhsT=fwb[r][:, tt, 0:1], rhs=Hs[:, tt, :], start=(tt == 0), stop=(tt == NTL - 1))), [f"fwb{r}", "Hs"], ["ps2"])
                            k = fc % 2
                            P.op("act", (lambda e, k=k, bk=bk: e.activation(out=go[k][:], in_=ps[bk][:, 0:CB], func=AF.Copy)), [f"ps{bk}"], [f"go{k}"])
                            if fc == NH:
                                P.op("dve", (lambda e, k=k: e.tensor_copy(out=go[k][0:1, :], in_=ps[2][0:1, 0:CB])), ["ps2", f"go{k}"], [f"go{k}"])
                            P.dma("sp", gfa_d[cbk, o, fc, :, :], go[k][:], [f"go{k}"], ["gfa_d"])
                    P.flush()

        NRI = 2
        hyb = hy_bias
        for cbk in range(NCB):
            c0 = cbk * CB
            for o in range(2):
                with contextlib.ExitStack() as ph:
                    dat = sb("dat", [128, NTL, CB], BF16, ph)
                    Ys = sb("Ys", [128, NF, CB], BF16, ph)
                    if o == 0:
                        load_tok_tiles(dat, u3_d, c0, "dat")
                    else:
                        load_tok_tiles(dat, z_d, c0, "dat")
                    with contextlib.ExitStack() as ph2:
                        fwb = [sb(f"fwc{i}", [128, NTL, 128], BF16, ph2) for i in range(NRF)]
                        gt_ = [sb(f"gt{i}", [128, 2, CB], F32, ph2) for i in range(2)]
                        tq = [[sb(f"tq{i}_{j}", [128, CB], F32, ph2) for j in range(4)] for i in range(2)]
                        order = []
                        for fc in range(NH):
                            order += [fc, NH + fc]
                        LA = NRF - 1
                        for it in range(NF + LA):
                            if it < NF:
                                r = it % NRF
                                P.dma("sp", fwb[r][:], fwr_d[order[it], :, :, :], [], [f"fwc{r}"])
                            if it >= LA:
                                idx = it - LA
                                fchunk = order[idx]
                                r = idx % NRF
                                fc = fchunk % NH
                                isb = fchunk >= NH
                                bk = (fc % 2) * 2 + (1 if isb else 0)
                                for tt in range(NTL):
                                    P.op("pe", (lambda e, r=r, tt=tt, bk=bk: e.matmul(ps[bk][:, 0:CB], lhsT=fwb[r][:, tt, :], rhs=dat[:, tt, :], start=(tt == 0), stop=(tt == NTL - 1))), [f"fwc{r}", "dat"], [f"ps{bk}"])
                                if isb:
                                    k = fc % 2
                                    pa, pb = (fc % 2) * 2, (fc % 2) * 2 + 1
                                    P.dma("act", gt_[k][:, 0, :], gfa_d[cbk, o, fc, :, :], ["gfa_d"], [f"gt{k}"])
                                    P.dma("act", gt_[k][:, 1, :], gfa_d[cbk, o, NH + fc, :, :], ["gfa_d"], [f"gt{k}"])
                                    T1, T2, T3, T4 = tq[k]
                                    rq = [f"tq{k}_{j}" for j in range(4)]
                                    P.op("dve", (lambda e, k=k, pa=pa, T1=T1: e.tensor_tensor(out=T1[:], in0=ps[pa][:, 0:CB], in1=gt_[k][:, 0, :], op=ALU.mult)), [f"ps{pa}", f"gt{k}"], [rq[0]])
                                    P.op("dve", (lambda e, k=k, pb=pb, T2=T2: e.tensor_tensor(out=T2[:], in0=ps[pb][:, 0:CB], in1=gt_[k][:, 1, :], op=ALU.mult)), [f"ps{pb}", f"gt{k}"], [rq[1]])
                                    P.op("dve", (lambda e, k=k, pa=pa, T3=T3: e.tensor_tensor(out=T3[:], in0=ps[pa][:, 0:CB], in1=gt_[k][:, 1, :], op=ALU.mult)), [f"ps{pa}", f"gt{k}"], [rq[2]])
                                    P.op("dve", (lambda e, k=k, pb=pb, T4=T4: e.tensor_tensor(out=T4[:], in0=ps[pb][:, 0:CB], in1=gt_[k][:, 0, :], op=ALU.mult)), [f"ps{pb}", f"gt{k}"], [rq[3]])
                                    P.op("pool", (lambda e, fc=fc, T1=T1, T2=T2: e.tensor_tensor(out=Ys[:, fc, :], in0=T1[:], in1=T2[:], op=ALU.subtract)), [rq[0], rq[1]], ["Ys"])
                                    P.op("pool", (lambda e, fc=fc, T3=T3, T4=T4: e.tensor_tensor(out=Ys[:, NH + fc, :], in0=T3[:], in1=T4[:], op=ALU.add)), [rq[2], rq[3]], ["Ys"])
                                    if fc == 0:
                                        P.op("pool", (lambda e, T1=T1: e.tensor_copy(out=Ys[0:1, 0, :], in_=T1[0:1, :])), [rq[0], "Ys"], ["Ys"])
                                        P.op("pool", (lambda e, T2=T2: e.tensor_copy(out=Ys[0:1, NH, :], in_=T2[0:1, :])), [rq[1], "Ys"], ["Ys"])
                        P.flush()
                    with contextlib.ExitStack() as ph2:
                        ivb = [sb(f"ivb{i}", [128, NF, 128], BF16, ph2) for i in range(NRI)]
                        bt = sb("bt", [128, CB], F32, ph2)
                        xt_ = [sb(f"xt{i}", [128, CB], BF16, ph2) for i in range(2)]
                        e1 = [sb(f"e1{i}", [128, CB], F32, ph2) for i in range(2)]
                        zo = [sb(f"zo{i}", [128, CB], BF16, ph2) for i in range(2)]
                        yo = [sb(f"yo{i}", [128, CB], F32, ph2) for i in range(2)]
                        ysb = [sb(f"ysb{i}", [128, CB // 128, 128], BF16, ph2) for i in range(2)]
                        P.dma("sp", bt[:], hyb[o, c0:c0 + CB].partition_broadcast(128), [], ["bt"])
                        xcol = (1 + o) * HYW + c0
                        mixv = mixT_d.rearrange("c p t -> p c t")
                        for it in range(NTL + 1):
                            if it < NTL:
                                r = it % NRI
                                P.dma("sp", ivb[r][:], ivr_d[it, :, :, :], [], [f"ivb{r}"])
                            if it >= 1:
                                tc_ = it - 1
                                r = tc_ % NRI
                                bk = tc_ % 2
                                k = tc_ % 2
                                P.dma("act", xt_[k][:], u3_d[tc_ * 128:(tc_ + 1) * 128, xcol:xcol + CB], ["u3_d"], [f"xt{k}"])
                                for fq in range(NF):
                                    P.op("pe", (lambda e, r=r, fq=fq, bk=bk: e.matmul(ps[bk][:, 0:CB], lhsT=ivb[r][:, fq, :], rhs=Ys[:, fq, :], start=(fq == 0), stop=(fq == NF - 1))), [f"ivb{r}", "Ys"], [f"ps{bk}"])
                                P.op("pool", (lambda e, k=k, tc_=tc_: e.tensor_tensor(out=e1[k][:], in0=dat[:, tc_, :], in1=bt[:], op=ALU.mult)), ["dat", "bt"], [f"e1{k}"])
                                P.op("dve", (lambda e, k=k, bk=bk: e.tensor_tensor(out=e1[k][:], in0=e1[k][:], in1=ps[bk][:, 0:CB], op=ALU.add)), [f"e1{k}", f"ps{bk}"], [f"e1{k}"])
                                if o == 0:
                                    P.op("pool", (lambda e, k=k: e.tensor_tensor(out=zo[k][:], in0=e1[k][:], in1=xt_[k][:], op=ALU.mult)), [f"e1{k}", f"xt{k}"], [f"zo{k}"])
                                    P.dma("sp", z_d[tc_ * 128:(tc_ + 1) * 128, c0:c0 + CB], zo[k][:], [f"zo{k}"], ["z_d"])
                                else:
                                    P.op("pool", (lambda e, k=k: e.tensor_tensor(out=yo[k][:], in0=e1[k][:], in1=xt_[k][:], op=ALU.mult)), [f"e1{k}", f"xt{k}"], [f"yo{k}"])
                                    pb2 = 4 + tc_ % 2
                                    for i4 in range(CB // 128):
                                        P.op("pe", (lambda e, k=k, i4=i4, pb2=pb2: e.transpose(out=ps[pb2][:, i4 * 128:(i4 + 1) * 128], in_=yo[k][:, i4 * 128:(i4 + 1) * 128], identity=ident)), [f"yo{k}", "cst"], [f"ps{pb2}"])
                                    P.op("act", (lambda e, k=k, pb2=pb2: e.activation(out=ysb[k][:].rearrange("p c t -> p (c t)"), in_=ps[pb2][:, 0:CB], func=AF.Copy)), [f"ps{pb2}"], [f"ysb{k}"])
                                    cc0 = c0 // 128
                                    P.dma("sp", mixv[:, cc0:cc0 + CB // 128, tc_ * 128:(tc_ + 1) * 128], ysb[k][:], [f"ysb{k}"], ["mixT_d"])
                        P.flush()

        if "stop_hy" in taps:
            return nc

        VC0 = PST0 + MLQK
        OC0 = PHY + MLQK
        with contextlib.ExitStack() as ph:
            AA = sb("AA", [128, NT, 8], F32, ph)
            EBB = sb("EBB", [128, NT, 8], F32, ph)
            EE = sb("EE", [128, NT, 8], F32, ph)
            gbb = sb("gbb", [128, 16], F32, ph)
            one_t = sb("one_t", [128, 1], F32, ph)
            P.dma("sp", gbb[:], ml_gb[0, :].partition_broadcast(128), [], ["gbb"])
            P.op("dve", lambda e: e.memset(one_t[:], 1.0), [], ["one_t"])
            gtl = [sb(f"gtl{i}", [128, 16], F32, ph) for i in range(2)]
            lp = [sb(f"lp{i}", [128, 8], F32, ph) for i in range(2)]
            igt = [sb(f"igt{i}", [128, 8], F32, ph) for i in range(2)]
            for c in range(NT):
                k = c % 2
                bk = c % 2
                G_, LP, IG = gtl[k], lp[k], igt[k]
                P.dma("sp", G_[:], g_d[c * 128:(c + 1) * 128, :], ["g_d"], [f"gtl{k}"])
                P.op("dve", (lambda e, G_=G_: e.tensor_tensor(out=G_[:], in0=G_[:], in1=gbb[:], op=ALU.add)), [f"gtl{k}", "gbb"], [f"gtl{k}"])
                for dr in range(2):
                    P.op("act", (lambda e, G_=G_, LP=LP, dr=dr: e.activation(out=LP[:, dr * 4:dr * 4 + 4], in_=G_[:, dr * 8 + 4:dr * 8 + 8], func=AF.Exp, scale=-1.0)), [f"gtl{k}"], [f"lp{k}"])
                    P.op("dve", (lambda e, G_=G_, IG=IG, dr=dr: e.tensor_copy(out=IG[:, dr * 4:dr * 4 + 4], in_=G_[:, dr * 8:dr * 8 + 4])), [f"gtl{k}"], [f"igt{k}"])
                P.op("act", (lambda e, LP=LP: e.activation(out=LP[:], in_=LP[:], func=AF.Ln, bias=one_t[:, 0:1], scale=1.0)), [f"lp{k}", "one_t"], [f"lp{k}"])
                P.op("pe", (lambda e, LP=LP, bk=bk: e.matmul(ps[bk][:, 0:4], lhsT=cst[:, C_MF, :], rhs=LP[:, 0:4], start=True, stop=True)), [f"lp{k}", "cst"], [f"ps{bk}"])
                P.op("pe", (lambda e, LP=LP, bk=bk: e.matmul(ps[bk][:, 4:8], lhsT=cst[:, C_MB, :], rhs=LP[:, 4:8], start=True, stop=True)), [f"lp{k}", "cst"], [f"ps{bk}"])
                P.op("pe", (lambda e, LP=LP, bk=bk: e.matmul(ps[bk][:, 8:16], lhsT=ones, rhs=LP[:, 0:8], start=True, stop=True)), [f"lp{k}", "cst"], [f"ps{bk}"])
                P.op("dve", (lambda e, IG=IG, bk=bk: e.tensor_tensor(out=IG[:], in0=IG[:], in1=ps[bk][:, 0:8], op=ALU.add)), [f"igt{k}", f"ps{bk}"], [f"igt{k}"])
                P.op("act", (lambda e, IG=IG, c=c: e.activation(out=AA[:, c, :], in_=IG[:], func=AF.Exp)), [f"igt{k}"], ["AA"])
                P.op("act", (lambda e, c=c, bk=bk: e.activation(out=EBB[:, c, :], in_=ps[bk][:, 0:8], func=AF.Exp)), [f"ps{bk}"], ["EBB"])
                P.op("act", (lambda e, c=c, bk=bk: e.activation(out=EE[:, c, :], in_=ps[bk][:, 8:16], func=AF.Exp, scale=-1.0)), [f"ps{bk}"], ["EE"])

            Chat = [[sb(f"Chat{d}_{h}", [128, NDC, DV], F32, ph) for h in range(H)] for d in range(2)]
            nhat = [[sb(f"nhat{d}_{h}", [128, NDC], F32, ph) for h in range(H)] for d in range(2)]
            Cb = [[sb(f"Cb{d}_{h}", [128, NDC, DV], BF16, ph) for h in range(H)] for d in range(2)]
            nb = [[sb(f"nb{d}_{h}", [128, NDC], BF16, ph) for h in range(H)] for d in range(2)]
            for d in range(2):
                for h in range(H):
                    P.op("pool", (lambda e, d=d, h=h: e.memset(Chat[d][h][:], 0.0)), [], [f"Chat{d}_{h}"])
                    P.op("pool", (lambda e, d=d, h=h: e.memset(nhat[d][h][:], 0.0)), [], [f"nhat{d}_{h}"])
                    P.op("pool", (lambda e, d=d, h=h: e.memset(Cb[d][h][:], 0.0)), [], [f"Cb{d}_{h}"])
                    P.op("pool", (lambda e, d=d, h=h: e.memset(nb[d][h][:], 0.0)), [], [f"nb{d}_{h}"])
            kt = [sb(f"kt{d}", [128, MLQK], BF16, ph) for d in range(2)]
            qt = [sb(f"qt{d}", [128, MLQK], BF16, ph) for d in range(2)]
            vt = [sb(f"vt{d}", [128, MLW], BF16, ph) for d in range(2)]
            kT = [sb(f"kT{d}", [128, H * NDC, 128], BF16, ph) for d in range(2)]
            qT = [sb(f"qT{d}", [128, H * NDC, 128], BF16, ph) for d in range(2)]
            VA = [sb(f"VA{i}", [128, DV], BF16, ph) for i in range(2)]
            acol = [sb(f"acol{i}", [128, 1], BF16, ph) for i in range(2)]
            STb = [sb(f"STb{i}", [128, 128], BF16, ph) for i in range(2)]
            rr = [sb(f"rr{i}", [128, 2], F32, ph) for i in range(2)]
            ho = [sb(f"ho{i}", [128, DV], F32, ph) for i in range(2)]
            order0 = list(range(NT))
            order1 = list(range(NTC - 1, -1, -1)) + list(range(NT - 1, NTC - 1, -1))
            orders = [order0, order1]
            psb = [ps[0][:].bitcast(BF16), ps[1][:].bitcast(BF16)]
            ucnt = 0
            NTR = H * NDC
            for step in range(NT):
                for d in range(2):
                    c = orders[d][step]
                    cprev = orders[d][step - 1] if step > 0 else c
                    lat = c >= NTC
                    P.dma("sp", kt[d][:], k_d[c * 128:(c + 1) * 128, :], ["k_d"], [f"kt{d}"])
                    P.dma("act", vt[d][:], p_d[c * 128:(c + 1) * 128, VC0:VC0 + MLW], ["p_d"], [f"vt{d}"])
                    if lat:
                        P.dma("sp", qt[d][:], q_d[(c - NTC) * 128:(c - NTC + 1) * 128, :], ["q_d"], [f"qt{d}"])
                        for (src, dstT, rs, rd) in ((kt[d], kT[d], f"kt{d}", f"kT{d}"), (qt[d], qT[d], f"qt{d}", f"qT{d}")):
                            for i0 in range(0, NTR, 8):
                                nn = min(8, NTR - i0)
                                bkT = (i0 // 8) % 2
                                for i in range(nn):
                                    P.op("pe", (lambda e, src=src, i=i, i0=i0, bkT=bkT: e.transpose(out=psb[bkT][:, i * 128:(i + 1) * 128], in_=src[:, (i0 + i) * 128:(i0 + i + 1) * 128], identity=cstb[:, C_ID, :])), [rs, "cstb"], [f"ps{bkT}"])
                                P.op("act", (lambda e, dstT=dstT, i0=i0, nn=nn, bkT=bkT: e.activation(out=dstT[:, i0:i0 + nn, :].rearrange("p c t -> p (c t)"), in_=psb[bkT][:, 0:nn * 128], func=AF.Copy)), [f"ps{bkT}"], [rd])
                    for h in range(H):
                        u = ucnt % 2
                        ucnt += 1
                        gcol = d * 4 + h
                        rC, rn_, rCb, rnb = f"Chat{d}_{h}", f"nhat{d}_{h}", f"Cb{d}_{h}", f"nb{d}_{h}"
                        P.op("dve", (lambda e, u=u, d=d, h=h, c=c, gcol=gcol: e.tensor_scalar(out=VA[u][:], in0=vt[d][:, h * DV:(h + 1) * DV], scalar1=AA[:, c, gcol:gcol + 1], scalar2=None, op0=ALU.mult)), [f"vt{d}", "AA"], [f"VA{u}"])
                        P.op("dve", (lambda e, u=u, c=c, gcol=gcol: e.tensor_copy(out=acol[u][:], in_=AA[:, c, gcol:gcol + 1])), ["AA"], [f"acol{u}"])
                        if lat:
                            bS = 2 + u
                            for dc in range(NDC):
                                P.op("pe", (lambda e, d=d, h=h, dc=dc, bS=bS: e.matmul(ps[bS][:, 0:128], lhsT=kT[d][:, h * NDC + dc, :], rhs=qT[d][:, h * NDC + dc, :], start=(dc == 0), stop=(dc == NDC - 1))), [f"kT{d}", f"qT{d}"], [f"ps{bS}"])
                            mk = C_MF if d == 0 else C_MB
                            P.op("dve", (lambda e, u=u, bS=bS, mk=mk: e.tensor_tensor(out=STb[u][:], in0=ps[bS][:, 0:128], in1=cst[:, mk, :], op=ALU.mult)), [f"ps{bS}", "cst"], [f"STb{u}"])
                            bN = 4 + u
                            P.op("pe", (lambda e, u=u, bN=bN: e.matmul(ps[bN][:, 0:DV], lhsT=STb[u][:], rhs=VA[u][:], start=True, stop=False)), [f"STb{u}", f"VA{u}"], [f"ps{bN}"])
                            for dc in range(NDC):
                                P.op("pe", (lambda e, d=d, h=h, dc=dc, bN=bN: e.matmul(ps[bN][:, 0:DV], lhsT=qT[d][:, h * NDC + dc, :], rhs=Cb[d][h][:, dc, :], start=False, stop=(dc == NDC - 1))), [f"qT{d}", rCb], [f"ps{bN}"])
                            P.op("pe", (lambda e, u=u, bS=bS: e.matmul(ps[bS][:, 256:257], lhsT=STb[u][:], rhs=acol[u][:], start=True, stop=False)), [f"STb{u}", f"acol{u}"], [f"ps{bS}"])
                            for dc in range(NDC):
                                P.op("pe", (lambda e, d=d, h=h, dc=dc, bS=bS: e.matmul(ps[bS][:, 256:257], lhsT=qT[d][:, h * NDC + dc, :], rhs=nb[d][h][:, dc:dc + 1], start=False, stop=(dc == NDC - 1))), [f"qT{d}", rnb], [f"ps{bS}"])
                            P.op("act", (lambda e, u=u, bS=bS: e.activation(out=rr[u][:, 0:1], in_=ps[bS][:, 256:257], func=AF.Abs)), [f"ps{bS}"], [f"rr{u}"])
                            P.op("dve", (lambda e, u=u, c=c, gcol=gcol: e.tensor_tensor(out=rr[u][:, 0:1], in0=rr[u][:, 0:1], in1=EBB[:, c, gcol:gcol + 1], op=ALU.max)), [f"rr{u}", "EBB"], [f"rr{u}"])
                            P.op("dve", (lambda e, u=u: e.reciprocal(out=rr[u][:, 1:2], in_=rr[u][:, 0:1])), [f"rr{u}"], [f"rr{u}"])
                            P.op("act", (lambda e, u=u, bN=bN: e.activation(out=ho[u][:], in_=ps[bN][:, 0:DV], func=AF.Copy, scale=rr[u][:, 1:2])), [f"ps{bN}", f"rr{u}"], [f"ho{u}"])
                            P.dma("sp", hf_d[d, (c - NTC) * 128:(c - NTC + 1) * 128, h * DV:(h + 1) * DV], ho[u][:], [f"ho{u}"], ["hf_d"])
                        for dc in range(NDC):
                            bU = 6 + dc % 2
                            P.op("pe", (lambda e, d=d, h=h, dc=dc, u=u, bU=bU: e.matmul(ps[bU][:, 0:DV], lhsT=kt[d][:, h * DQK + dc * 128:h * DQK + (dc + 1) * 128], rhs=VA[u][:], start=True, stop=True)), [f"kt{d}", f"VA{u}"], [f"ps{bU}"])
                            P.op("pe", (lambda e, d=d, h=h, dc=dc, u=u, bU=bU: e.matmul(ps[bU][:, DV:DV + 1] if DV < 512 else ps[2 + u][:, 300 + dc:301 + dc], lhsT=kt[d][:, h * DQK + dc * 128:h * DQK + (dc + 1) * 128], rhs=acol[u][:], start=True, stop=True)), [f"kt{d}", f"acol{u}"], [f"ps{bU}", f"ps{2 + u}"])
                            nps = (lambda dc=dc, bU=bU, u=u: ps[bU][:, DV:DV + 1] if DV < 512 else ps[2 + u][:, 300 + dc:301 + dc])
                            P.op("dve", (lambda e, d=d, h=h, dc=dc, bU=bU, cprev=cprev, gcol=gcol: e.scalar_tensor_tensor(out=Chat[d][h][:, dc, :], in0=Chat[d][h][:, dc, :], scalar=EE[:, cprev, gcol:gcol + 1], in1=ps[bU][:, 0:DV], op0=ALU.mult, op1=ALU.add)), [rC, "EE", f"ps{bU}"], [rC])
                            P.op("dve", (lambda e, d=d, h=h, dc=dc, cprev=cprev, gcol=gcol, nps=nps: e.scalar_tensor_tensor(out=nhat[d][h][:, dc:dc + 1], in0=nhat[d][h][:, dc:dc + 1], scalar=EE[:, cprev, gcol:gcol + 1], in1=nps(), op0=ALU.mult, op1=ALU.add)), [rn_, "EE", f"ps{bU}", f"ps{2 + u}"], [rn_])
                            P.op("act", (lambda e, d=d, h=h, dc=dc, c=c, gcol=gcol: e.activation(out=Cb[d][h][:, dc, :], in_=Chat[d][h][:, dc, :], func=AF.Copy, scale=EE[:, c, gcol:gcol + 1])), [rC, "EE"], [rCb])
                            P.op("act", (lambda e, d=d, h=h, dc=dc, c=c, gcol=gcol: e.activation(out=nb[d][h][:, dc:dc + 1], in_=nhat[d][h][:, dc:dc + 1], func=AF.Copy, scale=EE[:, c, gcol:gcol + 1])), [rn_, "EE"], [rnb])
            P.flush()

        with contextlib.ExitStack() as ph:
            NWb = sb("NWb", [128, MLW], F32, ph)
            P.dma("sp", NWb[:], ml_nw[0, :].partition_broadcast(128), [], ["NWb"])
            hA = [sb(f"hA{i}", [128, MLW], F32, ph) for i in range(2)]
            hB = [sb(f"hB{i}", [128, MLW], F32, ph) for i in range(2)]
            ob = [sb(f"ob{i}", [128, MLW], BF16, ph) for i in range(2)]
            og = [sb(f"og{i}", [128, MLW], F32, ph) for i in range(2)]
            st2 = [sb(f"st2{i}", [128, H, 6], F32, ph) for i in range(2)]
            mv2 = [sb(f"mv2{i}", [128, H, 4], F32, ph) for i in range(2)]
            msb = [sb(f"msb{i}", [128, MLW // 128, 128], BF16, ph) for i in range(2)]
            mixv = mixT_d.rearrange("c p t -> p c t")
            for tl_ in range(NTL):
                k = tl_ % 2
                A_, B_, O_, OG, S2, M2, MS = hA[k], hB[k], ob[k], og[k], st2[k], mv2[k], msb[k]
                P.dma("sp", A_[:], hf_d[0, tl_ * 128:(tl_ + 1) * 128, :], ["hf_d"], [f"hA{k}"])
                P.dma("act", B_[:], hf_d[1, tl_ * 128:(tl_ + 1) * 128, :], ["hf_d"], [f"hB{k}"])
                P.dma("sp", O_[:], p_d[(NTC + tl_) * 128:(NTC + tl_ + 1) * 128, OC0:OC0 + MLW], ["p_d"], [f"ob{k}"])
                P.op("pool", (lambda e, A_=A_, B_=B_: e.tensor_tensor(out=A_[:], in0=A_[:], in1=B_[:], op=ALU.add)), [f"hA{k}", f"hB{k}"], [f"hA{k}"])
                P.op("act", (lambda e, O_=O_, OG=OG: e.activation(out=OG[:], in_=O_[:], func=AF.Sigmoid)), [f"ob{k}"], [f"og{k}"])
                P.op("pool", (lambda e, OG=OG: e.tensor_tensor(out=OG[:], in0=OG[:], in1=NWb[:], op=ALU.mult)), [f"og{k}", "NWb"], [f"og{k}"])
                for h in range(H):
                    P.op("dve", (lambda e, A_=A_, S2=S2, h=h: e.bn_stats(out=S2[:, h, :], in_=A_[:, h * DV:(h + 1) * DV])), [f"hA{k}"], [f"st2{k}"])
                    P.op("dve", (lambda e, S2=S2, M2=M2, h=h: e.bn_aggr(out=M2[:, h, 0:2], in_=S2[:, h, :])), [f"st2{k}"], [f"mv2{k}"])
                    P.op("act", (lambda e, M2=M2, h=h: e.activation(out=M2[:, h, 2:3], in_=M2[:, h, 1:2], func=AF.Sqrt, bias=epst[:, 0:1], scale=1.0)), [f"mv2{k}", "epst"], [f"mv2{k}"])
                    P.op("dve", (lambda e, M2=M2, h=h: e.reciprocal(out=M2[:, h, 2:3], in_=M2[:, h, 2:3])), [f"mv2{k}"], [f"mv2{k}"])
                    P.op("dve", (lambda e, M2=M2, h=h: e.scalar_tensor_tensor(out=M2[:, h, 3:4], in0=M2[:, h, 0:1], scalar=-1.0, in1=M2[:, h, 2:3], op0=ALU.mult, op1=ALU.mult)), [f"mv2{k}"], [f"mv2{k}"])
                    P.op("act", (lambda e, A_=A_, M2=M2, h=h: e.activation(out=A_[:, h * DV:(h + 1) * DV], in_=A_[:, h * DV:(h + 1) * DV], func=AF.Identity, bias=M2[:, h, 3:4], scale=M2[:, h, 2:3])), [f"hA{k}", f"mv2{k}"], [f"hA{k}"])
                P.op("pool", (lambda e, A_=A_, OG=OG: e.tensor_tensor(out=A_[:], in0=A_[:], in1=OG[:], op=ALU.mult)), [f"hA{k}", f"og{k}"], [f"hA{k}"])
                for i0 in range(0, MLW // 128, 4):
                    bk = (i0 // 4) % 2
                    for i in range(4):
                        P.op("pe", (lambda e, A_=A_, i=i, i0=i0, bk=bk: e.transpose(out=ps[bk][:, i * 128:(i + 1) * 128], in_=A_[:, (i0 + i) * 128:(i0 + i + 1) * 128], identity=ident)), [f"hA{k}", "cst"], [f"ps{bk}"])
                    P.op("act", (lambda e, MS=MS, i0=i0, bk=bk: e.activation(out=MS[:, i0:i0 + 4, :].rearrange("p c t -> p (c t)"), in_=ps[bk][:], func=AF.Copy)), [f"ps{bk}"], [f"msb{k}"])
                cc0 = HYW // 128
                P.dma("sp", mixv[:, cc0:cc0 + MLW // 128, tl_ * 128:(tl_ + 1) * 128], MS[:], [f"msb{k}"], ["mixT_d"])
            P.flush()

        if "stop_ml" in taps:
            return nc

        latg = [list(range(a, min(a + 4, NOWN))) for a in range(0, NOWN, 4)]
        gemm_tok("wout", mixT_d, DC, w_out, D, latg, gated_epi(lambda gt: gt * 128), gated_setup({gt: 2 for gt in range(NOWN)}))
        tiles = []
        for t_ in range(NOWN):
            tiles.append(dict(src_v=vd_d[t_ * 128:(t_ + 1) * 128, :], src_x=x1_d[(NTC + t_) * 128:(NTC + t_ + 1) * 128, :],
                              dst_x=x2_d[t_ * 128:(t_ + 1) * 128, :], dstT=t_ * 128, r=0, s=2))
        ln_phase("ln2", tiles, cfg.ALPHA, ln_g[1, :], ln_b[1, :])

        nsg = 2 if NOWN > 17 else 1
        per = (NOWN + nsg - 1) // nsg
        for sgi in range(nsg):
            t0 = sgi * per
            t1 = min(NOWN, t0 + per)
            gemm_a(wi[1], t0 * 128, (t1 - t0) * 128)
        gemm_tok("ffn2b", hT_d, FC, wo[1], D, latg, gated_epi(lambda gt: gt * 128), gated_setup({gt: 3 for gt in range(NOWN)}))
        tiles = []
        for t_ in range(NOWN):
            tiles.append(dict(src_v=vd_d[t_ * 128:(t_ + 1) * 128, :], src_x=x2_d[t_ * 128:(t_ + 1) * 128, :],
                              dst_x=out_d[t_ * 128:(t_ + 1) * 128, :]))
        ln_phase("ln3", tiles, cfg.ALPHA, ln_g[2, :], ln_b[2, :])
    return nc


def prep_inputs(cfg, inp, b, consts, flip=False):
    D, DC = cfg.D, cfg.DC
    f32 = lambda a: np.ascontiguousarray(np.asarray(a, dtype=np.float32))
    A = lambda k: np.asarray(inp[k])
    cv = np.stack([A("c")[b], A("c_ctx")], axis=0)
    cvT = cv.reshape(2, DC, 128).transpose(2, 1, 0)
    ctxb, xb = A("ctx")[b], A("x")[b]
    w_in, gate_b = A("w_in")[0], A("ml_gate_b")[0]
    hcw, mcw, w3 = A("hy_conv_w")[0], A("ml_conv_w")[0], A("hy_filt_w3")[0]
    if flip:
        ctxb, xb = ctxb[::-1], xb[::-1]
        hcw, mcw = hcw[::-1], mcw[::-1]
        gperm = np.concatenate([np.arange(8, 16), np.arange(0, 8)])
        w_in = np.concatenate([w_in[:, :cfg.PT - 16], w_in[:, cfg.PT - 16:][:, gperm]], axis=1)
        gate_b = gate_b[gperm]
        w3 = w3.reshape(cfg.FH, 2, 2, cfg.HYW)[:, :, ::-1, :].reshape(cfg.FH, 4 * cfg.HYW)
    m = {
        "xin": f32(np.concatenate([ctxb, xb], axis=0)),
        "cvT": f32(cvT),
        "ada_w": f32(A("ada_w")[0]),
        "ada_bT": f32(A("ada_b")[0].reshape(9 * DC, 128).T),
        "ln_g": f32(A("ln_g")[0]), "ln_b": f32(A("ln_b")[0]),
        "ffn1_wi": f32(A("ffn1_wi")[0]), "ffn1_wo": f32(A("ffn1_wo")[0]),
        "ffn2_wi": f32(A("ffn2_wi")[0]), "ffn2_wo": f32(A("ffn2_wo")[0]),
        "w_in": f32(w_in),
        "hy_conv_w": f32(hcw), "hy_conv_b": f32(A("hy_conv_b")[0][None]),
        "hy_filt_w1": f32(A("hy_filt_w1")[0]), "hy_filt_b1": f32(A("hy_filt_b1")[0][:, None]),
        "hy_filt_f1": f32(A("hy_filt_f1")[0][:, None]),
        "hy_filt_w2": f32(A("hy_filt_w2")[0]), "hy_filt_b2": f32(A("hy_filt_b2")[0][:, None]),
        "hy_filt_f2": f32(A("hy_filt_f2")[0][:, None]),
        "hy_filt_w3": f32(w3), "hy_bias": f32(A("hy_bias")[0]),
        "ml_conv_w": f32(mcw), "ml_conv_b": f32(A("ml_conv_b")[0][None]),
        "ml_gate_b": f32(gate_b[None]), "ml_norm_w": f32(A("ml_norm_w")[0][None]),
        "w_out": f32(A("w_out")[0]),
    }
    m.update(consts)
    return m


def kernel(**inputs):
    cfg = Cfg()
    B = np.asarray(inputs["x"]).shape[0]
    consts = host_consts(cfg)
    half = cfg.NTL // 2
    nc = build(cfg, nown=half)
    shared = {}
    in_maps = []
    for b in range(B):
        for r in range(2):
            m = prep_inputs(cfg, inputs, b, consts, flip=(r == 1))
            for k in list(m.keys()):
                if k in ("xin", "cvT"):
                    continue
                key = (k, r if k in ("w_in", "hy_conv_w", "ml_conv_w", "ml_gate_b", "hy_filt_w3") else 0)
                if key in shared:
                    m[k] = shared[key]
                else:
                    shared[key] = m[k]
            in_maps.append(m)
    res = run_bass_kernel_spmd(nc, in_maps, core_ids=list(range(2 * B)))
    out = np.empty((B, cfg.L, cfg.D), np.float32)
    hl = half * 128
    for b in range(B):
        out[b, :hl] = np.asarray(res.results[2 * b]["out"], dtype=np.float32)
        out[b, hl:] = np.asarray(res.results[2 * b + 1]["out"], dtype=np.float32)[::-1]
    return out
```

```python
import contextlib
import math
import numpy as np
import ml_dtypes
import concourse.bass as bass
import concourse.mybir as mybir
from concourse.bass_utils import run_bass_kernel_spmd

F32 = mybir.dt.float32
BF16 = mybir.dt.bfloat16
AF = mybir.ActivationFunctionType
ALU = mybir.AluOpType
LN_EPS = 1e-5
INTERLEAVE_ADA = False
LN_NB = 2
RECYCLE_SEMS = False


class Cfg:
    def __init__(s, D=4096, L=4096, CL=256, F=11008, GW=64):
        s.D, s.L, s.CL, s.F, s.GW = D, L, CL, F, GW
        s.DC = D // 128
        s.FC = F // 128
        s.HYW = D // 2
        s.MLW = D - s.HYW
        s.H = 4
        s.DV = s.MLW // 4
        s.DQK = s.DV // 2
        s.MLQK = 4 * s.DQK
        s.NDC = s.DQK // 128
        s.NG = 16
        s.PHY = 3 * s.HYW
        s.PST0 = s.PHY + s.MLQK + s.MLW
        s.PT = s.PST0 + s.MLQK + s.MLW + s.NG
        s.T = L + CL
        s.NT = s.T // 128
        s.NTC = CL // 128
        s.NTL = L // 128
        s.ALPHA = 2.0 ** 0.25
        s.FH = 64
        s.EMB = 33
        s.CB = 512
        s.NCB = s.HYW // s.CB
        s.NF = 2 * L // 128


class _Op:
    __slots__ = ("eng", "fn", "reads", "writes", "dma", "idx", "ms", "sem", "val")

    def __init__(s, eng, fn, reads, writes, dma):
        s.eng, s.fn, s.reads, s.writes, s.dma = eng, fn, tuple(reads), tuple(writes), dma
        s.ms = False
        s.sem = None
        s.val = 0


class Prog:
    def __init__(s, nc, es):
        s.nc, s.es = nc, es
        s.ops = []
        s.engobj = {"pe": nc.tensor, "act": nc.scalar, "dve": nc.vector, "pool": nc.gpsimd, "sp": nc.sync}
        s.engsem, s.engcnt, s.ressem, s.rescnt, s.waited = {}, {}, {}, {}, {}
        s.nsem = 0
        s.nins = 0
        s.freesems = []
        s.maxcnt = 0

    def op(s, eng, fn, reads=(), writes=(), dma=False):
        o = _Op(eng, fn, reads, writes, dma)
        o.idx = len(s.ops)
        s.ops.append(o)
        return o

    def dma(s, eng, out, in_, reads, writes):
        assert len(writes) == 1
        return s.op(eng, lambda e: e.dma_start(out=out, in_=in_), reads, writes, dma=True)

    def _newsem(s):
        s.nsem += 1
        return s.es.enter_context(s.nc.semaphore(f"sm{s.nsem}"))

    def flush(s):
        ops = s.ops
        if not ops:
            return
        lastw, rd_eng, rd_dma = {}, {}, {}
        deps = []
        for o in ops:
            d = set()
            for r in o.reads:
                if r in lastw:
                    d.add(lastw[r])
            for w in o.writes:
                if w in lastw:
                    d.add(lastw[w])
                for i in rd_eng.get(w, {}).values():
                    d.add(i)
                for i in rd_dma.get(w, ()):
                    d.add(i)
            d.discard(o.idx)
            deps.append(d)
            for w in o.writes:
                lastw[w] = o.idx
                rd_eng[w] = {}
                rd_dma[w] = []
            for r in o.reads:
                if r in o.writes:
                    continue
                if o.dma:
                    rd_dma.setdefault(r, []).append(o.idx)
                else:
                    rd_eng.setdefault(r, {})[o.eng] = o.idx
        lastop = {}
        for o in ops:
            if o.dma:
                o.ms = True
            else:
                lastop[o.eng] = o
            for di in deps[o.idx]:
                p = ops[di]
                if p.dma:
                    continue
                if p.eng == "pe" and o.eng == "pe" and not o.dma:
                    continue
                p.ms = True
        for o in lastop.values():
            o.ms = True
        for o in ops:
            if not o.ms:
                continue
            if o.dma:
                r = o.writes[0]
                if r not in s.ressem:
                    pick = None
                    for i_, (sm_, c_) in enumerate(s.freesems):
                        if c_ <= 8000:
                            pick = i_
                            break
                    if pick is not None:
                        s.ressem[r], s.rescnt[r] = s.freesems.pop(pick)
                    else:
                        s.ressem[r] = s._newsem()
                        s.rescnt[r] = 0
                s.rescnt[r] += 16
                s.maxcnt = max(s.maxcnt, s.rescnt[r])
                o.sem, o.val = s.ressem[r], s.rescnt[r]
            else:
                e = o.eng
                if e not in s.engsem or s.engcnt[e] >= 30000:
                    s.engsem[e] = s._newsem()
                    s.engcnt[e] = 0
                s.engcnt[e] += 1
                o.sem, o.val = s.engsem[e], s.engcnt[e]
        for o in ops:
            e = s.engobj[o.eng]
            need = {}
            for di in deps[o.idx]:
                p = ops[di]
                if p.sem is None:
                    continue
                k = id(p.sem)
                if k not in need or need[k][1] < p.val:
                    need[k] = (p.sem, p.val)
            for k, (sem, val) in need.items():
                wk = (o.eng, k)
                if s.waited.get(wk, 0) >= val:
                    continue
                s.waited[wk] = val
                e.wait_ge(sem, val)
            ins = o.fn(e)
            s.nins += 1
            if o.ms:
                ins.then_inc(o.sem, 16 if o.dma else 1)
        for en, e in s.engobj.items():
            for e2, sem in s.engsem.items():
                if e2 == en:
                    continue
                wk = (en, id(sem))
                if s.waited.get(wk, 0) < s.engcnt[e2]:
                    s.waited[wk] = s.engcnt[e2]
                    e.wait_ge(sem, s.engcnt[e2])
            for r, sem in s.ressem.items():
                wk = (en, id(sem))
                if s.waited.get(wk, 0) < s.rescnt[r]:
                    s.waited[wk] = s.rescnt[r]
                    e.wait_ge(sem, s.rescnt[r])
        if RECYCLE_SEMS:
            for r, sem in s.ressem.items():
                s.freesems.append((sem, s.rescnt[r]))
            s.ressem, s.rescnt = {}, {}
        s.ops = []


C_ID, C_ONES, C_MF, C_MB, C_SL0, C_SL2, C_SC0, C_SC2, C_EUP, C_EDN = range(10)
NCST = 10


def host_consts(cfg):
    i = np.arange(128)
    s_, t_ = i[:, None], i[None, :]
    c = np.zeros((128, NCST, 128), np.float32)
    c[:, C_ID] = (s_ == t_)
    c[:, C_ONES] = 1.0
    c[:, C_MF] = (s_ <= t_)
    c[:, C_MB] = (s_ >= t_)
    c[:, C_SL0] = (s_ == t_ - 1) & (t_ % cfg.GW != 0)
    c[:, C_SL2] = (s_ == t_ + 1) & (t_ % cfg.GW != cfg.GW - 1)
    c[:, C_SC0] = (s_ == t_ - 1)
    c[:, C_SC2] = (s_ == t_ + 1)
    c[:, C_EUP] = (s_ == 0) & (t_ == 127)
    c[:, C_EDN] = (s_ == 127) & (t_ == 0)
    L = cfg.L
    t = np.arange(L, dtype=np.float64)[:, None]
    f = np.arange(L, dtype=np.float64)[None, :]
    ang = 2.0 * np.pi * ((t * f) % (2 * L)) / (2 * L)
    fw = np.concatenate([np.cos(ang), np.sin(ang)], axis=1)
    fw[:, L] = np.where(np.arange(L) % 2 == 0, 1.0, -1.0)
    NF, NTL = cfg.NF, cfg.NTL
    fwr = fw.reshape(NTL, 128, NF, 128).transpose(2, 1, 0, 3)
    fwr = np.ascontiguousarray(fwr).astype(ml_dtypes.bfloat16)
    wgt = np.full((2 * L,), 1.0 / L)
    wgt[0] = 0.5 / L
    wgt[L] = 0.5 / L
    iv = (fw * wgt[None, :]).T
    ivr = iv.reshape(NF, 128, NTL, 128).transpose(2, 1, 0, 3)
    ivr = np.ascontiguousarray(ivr).astype(ml_dtypes.bfloat16)
    tl = np.linspace(0.0, 1.0, L, dtype=np.float32)[:, None]
    w = (2.0 * math.pi / L) * np.arange(L, dtype=np.float32)[:, None]
    bands = np.linspace(1e-4, 15.0, 16, dtype=np.float32)[None, :]
    z = np.concatenate([tl, np.cos(bands * w), -np.sin(bands * w)], axis=-1).astype(np.float32)
    zT = np.ascontiguousarray(z.T)
    mx = math.log(1e-2) / 0.3
    mn = math.log(1e-2) / 1.5
    deltas = np.abs(np.linspace(mn, mx, cfg.HYW, dtype=np.float32)).astype(np.float32)[None, :]
    negt = np.ascontiguousarray((-tl[:, 0]).astype(np.float32).reshape(cfg.NTL, 128).T)
    return {"cst": c, "cstb": c.astype(ml_dtypes.bfloat16), "fwr": fwr, "ivr": ivr, "zT": zT,
            "deltas": np.ascontiguousarray(deltas), "negt": np.ascontiguousarray(negt)}


def build(cfg, taps=(), nown=None):
    nc = bass.Bass("TRN2", target_bir_lowering=False)
    D, L, CL, F, T = cfg.D, cfg.L, cfg.CL, cfg.F, cfg.T
    DC, FC, NT, NTC, NTL = cfg.DC, cfg.FC, cfg.NT, cfg.NTC, cfg.NTL
    HYW, MLW, MLQK, DV, DQK, NDC, H = cfg.HYW, cfg.MLW, cfg.MLQK, cfg.DV, cfg.DQK, cfg.NDC, cfg.H
    PHY, PST0, PT = cfg.PHY, cfg.PST0, cfg.PT
    NOWN = NTL if nown is None else nown

    def din(name, shape, dt=F32):
        return nc.dram_tensor(name, list(shape), dt, kind="ExternalInput").ap()

    def dscr(name, shape, dt):
        kind = "ExternalOutput" if name in taps else "Internal"
        return nc.dram_tensor(name, list(shape), dt, kind=kind).ap()

    xin = din("xin", [T, D])
    cvT = din("cvT", [128, DC, 2])
    ada_w = din("ada_w", [D, 9 * D])
    ada_bT = din("ada_bT", [128, 9 * DC])
    ln_g = din("ln_g", [3, D])
    ln_b = din("ln_b", [3, D])
    wi = [din("ffn1_wi", [D, 2 * F]), din("ffn2_wi", [D, 2 * F])]
    wo = [din("ffn1_wo", [F, D]), din("ffn2_wo", [F, D])]
    w_in = din("w_in", [D, PT])
    hy_cw = din("hy_conv_w", [3, PHY])
    hy_cb = din("hy_conv_b", [1, PHY])
    fw1 = din("hy_filt_w1", [cfg.EMB, cfg.FH])
    fb1 = din("hy_filt_b1", [cfg.FH, 1])
    ff1 = din("hy_filt_f1", [cfg.FH, 1])
    fw2 = din("hy_filt_w2", [cfg.FH, cfg.FH])
    fb2 = din("hy_filt_b2", [cfg.FH, 1])
    ff2 = din("hy_filt_f2", [cfg.FH, 1])
    fw3 = din("hy_filt_w3", [cfg.FH, 4 * HYW])
    hy_bias = din("hy_bias", [2, HYW])
    ml_cw = din("ml_conv_w", [3, 2 * MLQK])
    ml_cb = din("ml_conv_b", [1, 2 * MLQK])
    ml_gb = din("ml_gate_b", [1, 16])
    ml_nw = din("ml_norm_w", [1, MLW])
    w_out = din("w_out", [D, D])
    cst_d = din("cst", [128, NCST, 128])
    cstb_d = din("cstb", [128, NCST, 128], BF16)
    fwr_d = din("fwr", [cfg.NF, 128, NTL, 128], BF16)
    ivr_d = din("ivr", [NTL, 128, cfg.NF, 128], BF16)
    zT_d = din("zT", [cfg.EMB, L])
    deltas_d = din("deltas", [1, HYW])
    negt_d = din("negt", [128, NTL])
    out_d = nc.dram_tensor("out", [NOWN * 128, D], F32, kind="ExternalOutput").ap()

    gb_d = dscr("gb_d", [4, 128, D], F32)
    x0_d = dscr("x0_d", [T, D], F32)
    x1_d = dscr("x1_d", [T, D], F32)
    x2_d = dscr("x2_d", [L, D], F32)
    vd_d = dscr("vd_d", [T, D], F32)
    aT_d = dscr("aT_d", [DC, 128, T], BF16)
    hT_d = dscr("hT_d", [FC, 128, T], BF16)
    p_d = dscr("p_d", [T, PT - 16], BF16)
    g_d = dscr("g_d", [T, 16], F32)
    u3_d = dscr("u3_d", [L, PHY], BF16)
    q_d = dscr("q_d", [L, MLQK], BF16)
    k_d = dscr("k_d", [T, MLQK], BF16)
    hw_d = dscr("hw_d", [L, 4 * HYW], F32)
    hsd_d = dscr("hsd_d", [2, 2, L, HYW], BF16)
    gf_d = dscr("gf_d", [cfg.NF, 128, cfg.CB], F32)
    z_d = dscr("z_d", [L, HYW], BF16)
    hf_d = dscr("hf_d", [2, L, MLW], F32)
    mixT_d = dscr("mixT_d", [DC, 128, L], BF16)

    es = contextlib.ExitStack()
    with es:
        P = Prog(nc, es)
        global LAST_PROG
        LAST_PROG = P
        _uid = [0]

        def sb(name, shape, dt=F32, st=es):
            _uid[0] += 1
            return st.enter_context(nc.sbuf_tensor(f"{name}_{_uid[0]}", list(shape), dt))
        ps = [es.enter_context(nc.psum_tensor(f"ps{i}", [128, 512], F32)) for i in range(8)]
        cst = sb("cst_s", [128, NCST, 128])
        cstb = sb("cstb_s", [128, NCST, 128], BF16)
        mT = sb("mT", [128, 2, 9 * DC])
        sc1 = sb("sc1", [128, 2, 3, DC])
        hg = sb("hg", [128, 2, 3, DC])
        epst = sb("epst", [128, 1])
        P.dma("sp", cst[:], cst_d[:, :, :], [], ["cst"])
        P.dma("sp", cstb[:], cstb_d[:, :, :], [], ["cstb"])
        P.op("dve", lambda e: e.memset(epst[:], LN_EPS), [], ["epst"])
        ident = cst[:, C_ID, :]
        ones = cst[:, C_ONES, :]

        def mvec(r, j):
            return mT[:, r, j * DC:(j + 1) * DC]

        def phase_a_and_ln0():
            with contextlib.ExitStack() as ph:
                sT = sb("sT", [128, DC, 2], F32, ph)
                bT = sb("bT", [128, 9 * DC], F32, ph)
                NR = 3
                ring = [sb(f"adar{i}", [128, DC, 128], F32, ph) for i in range(NR)]
                P.dma("sp", sT[:], cvT[:, :, :], [], ["sT"])
                P.dma("sp", bT[:], ada_bT[:, :], [], ["bT"])
                P.op("act", lambda e: e.activation(out=sT[:], in_=sT[:], func=AF.Silu), ["sT"], ["sT"])
                awv = ada_w.rearrange("(kc p) n -> p kc n", p=128)
                NCC = 9 * DC
                CPS = 3 * DC
                LA = NR - 1
                state = {"it": 0}

                def psv(s_):
                    return ps[2 + s_][:, 0:2 * CPS].rearrange("p (c r) -> p c r", r=2)

                def finish_sublayer(s_):
                    pst = psv(s_)
                    for r in range(2):
                        P.op("dve", (lambda e, s_=s_, r=r, pst=pst: e.tensor_tensor(out=mT[:, r, s_ * CPS:(s_ + 1) * CPS], in0=pst[:, 0:CPS, r], in1=bT[:, s_ * CPS:(s_ + 1) * CPS], op=ALU.add)),
                             [f"ps{2 + s_}", "bT"], ["mT"])
                    for r in range(2):
                        P.op("dve", (lambda e, r=r, s_=s_: e.tensor_scalar(out=sc1[:, r, s_, :], in0=mvec(r, 3 * s_ + 1), scalar1=1.0, scalar2=None, op0=ALU.add)), ["mT"], ["sc1"])
                        gsc = 1.0 if s_ == 1 else 0.5
                        P.op("dve", (lambda e, r=r, s_=s_, gsc=gsc: e.tensor_scalar(out=hg[:, r, s_, :], in0=mvec(r, 3 * s_ + 2), scalar1=gsc, scalar2=None, op0=ALU.mult)), ["mT"], ["hg"])

                def advance(nchunks):
                    for _ in range(nchunks):
                        it = state["it"]
                        if it >= NCC + LA:
                            return
                        state["it"] += 1
                        if it < NCC:
                            r = it % NR
                            P.dma("sp" if it % 2 == 0 else "act", ring[r][:], awv[:, :, it * 128:(it + 1) * 128], [], [f"adar{r}"])
                        if it >= LA:
                            cc = it - LA
                            r = cc % NR
                            s_, lc = cc // CPS, cc % CPS
                            pst = psv(s_)
                            for kc in range(DC):
                                P.op("pe", (lambda e, r=r, kc=kc, lc=lc, pst=pst: e.matmul(pst[:, lc, :], lhsT=ring[r][:, kc, :], rhs=sT[:, kc, :], start=(kc == 0), stop=(kc == DC - 1))),
                                     [f"adar{r}", "sT"], [f"ps{2 + s_}"])
                            if lc == CPS - 1:
                                finish_sublayer(s_)

                tiles0 = []
                for gt in range(NT):
                    r = 1 if gt < NTC else 0
                    tiles0.append(dict(src_v=xin[gt * 128:(gt + 1) * 128, :], dst_x=x0_d[gt * 128:(gt + 1) * 128, :], dstT=gt * 128, r=r, s=0))
                if INTERLEAVE_ADA:
                    advance(CPS + LA)
                    rest = NCC + LA - state["it"]
                    per_tile = (rest + NT - 1) // NT
                    ln_phase("ln0", tiles0, 1.0, None, None, psb=(0, 1), between=lambda i: advance(per_tile), do_flush=False)
                advance(NCC + LA)
                dg = [sb(f"dg{i}", [128, 128], F32, ph) for i in range(2)]
                gst = [sb(f"gst{i}", [128, 512], F32, ph) for i in range(2)]
                combos = [(0, 0), (1, 0), (0, 1), (0, 2)]
                cnt = 0
                for gi, (r, s_) in enumerate(combos):
                    for c4 in range(DC // 4):
                        bk = 6 + (cnt % 2)
                        for ci in range(4):
                            c = c4 * 4 + ci
                            k = (cnt * 4 + ci) % 2
                            P.op("dve", (lambda e, k=k, r=r, s_=s_, c=c: e.tensor_scalar(out=dg[k][:], in0=ident, scalar1=hg[:, r, s_, c:c + 1], scalar2=None, op0=ALU.mult)),
                                 ["hg", "cst"], [f"dg{k}"])
                            P.op("pe", (lambda e, k=k, bk=bk, ci=ci: e.matmul(ps[bk][:, ci * 128:(ci + 1) * 128], lhsT=ones, rhs=dg[k][:], start=True, stop=True)),
                                 [f"dg{k}", "cst"], [f"ps{bk}"])
                        k2 = cnt % 2
                        P.op("act", (lambda e, k2=k2, bk=bk: e.activation(out=gst[k2][:], in_=ps[bk][:], func=AF.Copy)), [f"ps{bk}"], [f"gst{k2}"])
                        P.dma("sp", gb_d[gi, :, c4 * 512:(c4 + 1) * 512], gst[k2][:], [f"gst{k2}"], ["gb_d"])
                        cnt += 1
                P.flush()
            if not INTERLEAVE_ADA:
                ln_phase("ln0", tiles0, 1.0, None, None)

        def ln_phase(tag, tiles, alpha, lng, lnb, psb=(0, 1), between=None, do_flush=True):
            with contextlib.ExitStack() as ph:
                NB = LN_NB
                has_x = any(tl.get("src_x") is not None for tl in tiles)
                vb = [sb(f"vb{i}", [128, D], F32, ph) for i in range(NB)]
                xb = [sb(f"xb{i}", [128, D], F32, ph) for i in range(NB)] if has_x else [None] * NB
                ust = [sb(f"ust{i}", [128, DC, 128], BF16, ph) for i in range(NB)]
                nch = D // 512
                st = [sb(f"st{i}", [128, nch, 6], F32, ph) for i in range(NB)]
                mv = [sb(f"mv{i}", [128, 4], F32, ph) for i in range(NB)]
                if lng is not None:
                    LNG = sb("LNG", [128, D], F32, ph)
                    LNB = sb("LNB", [128, D], F32, ph)
                    P.dma("sp", LNG[:], lng.partition_broadcast(128), [], ["LNG"])
                    P.dma("sp", LNB[:], lnb.partition_broadcast(128), [], ["LNB"])
                aTv = aT_d.rearrange("c p t -> p c t")
                for i, tl in enumerate(tiles):
                    b = i % NB
                    V, X, ST, MV, US = vb[b], xb[b], st[b], mv[b], ust[b]
                    rv, rx, rst, rmv, rus = f"vb{b}", f"xb{b}", f"st{b}", f"mv{b}", f"ust{b}"
                    P.dma("sp", V[:], tl["src_v"], ["vd_d", "xin"], [rv])
                    if tl.get("src_x") is not None:
                        P.dma("act", X[:], tl["src_x"], ["x0_d", "x1_d"], [rx])
                        P.op("dve", (lambda e, V=V, X=X: e.scalar_tensor_tensor(out=V[:], in0=X[:], scalar=alpha, in1=V[:], op0=ALU.mult, op1=ALU.add)), [rx, rv], [rv])
                    for c in range(nch):
                        P.op("dve", (lambda e, V=V, ST=ST, c=c: e.bn_stats(out=ST[:, c, :], in_=V[:, c * 512:(c + 1) * 512])), [rv], [rst])
                    P.op("dve", (lambda e, ST=ST, MV=MV: e.bn_aggr(out=MV[:, 0:2], in_=ST[:].rearrange("p c s -> p (c s)"))), [rst], [rmv])
                    P.op("act", (lambda e, MV=MV: e.activation(out=MV[:, 2:3], in_=MV[:, 1:2], func=AF.Sqrt, bias=epst[:, 0:1], scale=1.0)), [rmv, "epst"], [rmv])
                    P.op("dve", (lambda e, MV=MV: e.reciprocal(out=MV[:, 2:3], in_=MV[:, 2:3])), [rmv], [rmv])
                    P.op("dve", (lambda e, MV=MV: e.scalar_tensor_tensor(out=MV[:, 3:4], in0=MV[:, 0:1], scalar=-1.0, in1=MV[:, 2:3], op0=ALU.mult, op1=ALU.mult)), [rmv], [rmv])
                    P.op("act", (lambda e, V=V, MV=MV: e.activation(out=V[:], in_=V[:], func=AF.Identity, bias=MV[:, 3:4], scale=MV[:, 2:3])), [rv, rmv], [rv])
                    if lng is not None:
                        P.op("pool", (lambda e, V=V: e.tensor_tensor(out=V[:], in0=V[:], in1=LNG[:], op=ALU.mult)), [rv, "LNG"], [rv])
                        P.op("pool", (lambda e, V=V: e.tensor_tensor(out=V[:], in0=V[:], in1=LNB[:], op=ALU.add)), [rv, "LNB"], [rv])
                    if tl.get("dst_x") is not None:
                        P.dma("sp", tl["dst_x"], V[:], [rv], ["dstx"])
                    if between is not None:
                        between(i)
                    if tl.get("dstT") is not None:
                        r, s_ = tl["r"], tl["s"]
                        for c in range(DC):
                            bk = psb[(c // 4) % 2]
                            P.op("pe", (lambda e, V=V, c=c, bk=bk: e.transpose(out=ps[bk][:, (c % 4) * 128:(c % 4 + 1) * 128], in_=V[:, c * 128:(c + 1) * 128], identity=ident)),
                                 [rv, "cst"], [f"ps{bk}"])
                            P.op("act", (lambda e, US=US, c=c, bk=bk, r=r, s_=s_: e.activation(out=US[:, c, :], in_=ps[bk][:, (c % 4) * 128:(c % 4 + 1) * 128], func=AF.Identity,
                                                                                    bias=mvec(r, 3 * s_)[:, c:c + 1], scale=sc1[:, r, s_, c:c + 1])),
                                 [f"ps{bk}", "mT", "sc1"], [rus])
                        t0 = tl["dstT"]
                        P.dma("sp", aTv[:, :, t0:t0 + 128], US[:], [rus], ["aT_d"])
                if do_flush:
                    P.flush()

        def gemm_a(Wi, tok0, Tsg):
            with contextlib.ExitStack() as ph:
                xT = sb("xT", [128, DC, Tsg], BF16, ph)
                NR = 3
                wg = [sb(f"wg{i}", [128, DC, 128], BF16, ph) for i in range(NR)]
                wu = [sb(f"wu{i}", [128, DC, 128], BF16, ph) for i in range(NR)]
                hb = [sb(f"hb{i}", [128, Tsg], BF16, ph) for i in range(2)]
                sg = [sb(f"sg{i}", [128, 512], F32, ph) for i in range(2)]
                aTv = aT_d.rearrange("c p t -> p c t")
                q4 = max(1, DC // 4)
                for a in range(0, DC, q4):
                    P.dma("sp", xT[:, a:a + q4, :], aTv[:, a:a + q4, tok0:tok0 + Tsg], ["aT_d"], ["xT"])
                groups = [(t0, min(512, Tsg - t0)) for t0 in range(0, Tsg, 512)]
                wv = Wi.rearrange("(kc p) n -> p kc n", p=128)
                hTv = hT_d
                LA = NR - 1
                gcnt = 0
                for it in range(FC + LA):
                    if it < FC:
                        r = it % NR
                        P.dma("pool", wg[r][:], wv[:, :, it * 128:(it + 1) * 128], [], [f"wg{r}"])
                        P.dma("pool", wu[r][:], wv[:, :, F + it * 128:F + (it + 1) * 128], [], [f"wu{r}"])
                    if it >= LA:
                        j = it - LA
                        r = j % NR
                        hbj = hb[j % 2]
                        for (t0, n) in groups:
                            pg, pu = (gcnt % 4) * 2, (gcnt % 4) * 2 + 1
                            k = gcnt % 2
                            gcnt += 1
                            for kc in range(DC):
                                P.op("pe", (lambda e, r=r, kc=kc, pg=pg, t0=t0, n=n: e.matmul(ps[pg][:, 0:n], lhsT=wg[r][:, kc, :], rhs=xT[:, kc, t0:t0 + n], start=(kc == 0), stop=(kc == DC - 1))),
                                     [f"wg{r}", "xT"], [f"ps{pg}"])
                            for kc in range(DC):
                                P.op("pe", (lambda e, r=r, kc=kc, pu=pu, t0=t0, n=n: e.matmul(ps[pu][:, 0:n], lhsT=wu[r][:, kc, :], rhs=xT[:, kc, t0:t0 + n], start=(kc == 0), stop=(kc == DC - 1))),
                                     [f"wu{r}", "xT"], [f"ps{pu}"])
                            P.op("act", (lambda e, k=k, pg=pg, n=n: e.activation(out=sg[k][:, 0:n], in_=ps[pg][:, 0:n], func=AF.Silu)), [f"ps{pg}"], [f"sg{k}"])
                            P.op("dve", (lambda e, k=k, pu=pu, n=n, t0=t0, hbj=hbj: e.tensor_tensor(out=hbj[:, t0:t0 + n], in0=sg[k][:, 0:n], in1=ps[pu][:, 0:n], op=ALU.mult)),
                                 [f"sg{k}", f"ps{pu}"], [f"hb{j % 2}"])
                        P.dma("sp", hTv[j, :, tok0:tok0 + Tsg], hbj[:], [f"hb{j % 2}"], ["hT_d"])
                P.flush()

        def gemm_tok(tag, aTd, KC, Wd, Ncols, tgroups, epi, extra_setup=None, col_filter=None):
            with contextlib.ExitStack() as ph:
                KP = 8
                NR = 4
                aT = [sb(f"aT{i}", [128, KC, 512], BF16, ph) for i in range(1)]
                wr = [sb(f"wr{i}", [128, KP, 512], BF16, ph) for i in range(NR)]
                state = extra_setup(ph) if extra_setup is not None else None
                aTv = aTd.rearrange("c p t -> p c t")
                wv = Wd.rearrange("(j p) n -> p j n", p=128)
                ncolt = (Ncols + 511) // 512
                pieces = [(j0, min(KP, KC - j0)) for j0 in range(0, KC, KP)]
                par = 0
                for g in tgroups:
                    ntok = 128 * len(g)
                    tok0 = g[0] * 128
                    assert all(g[i] == g[0] + i for i in range(len(g)))
                    q4 = max(1, KC // 4)
                    for a in range(0, KC, q4):
                        a1 = min(KC, a + q4)
                        P.dma("sp", aT[0][:, a:a1, 0:ntok], aTv[:, a:a1, tok0:tok0 + ntok], [], ["aT0"])
                    work = []
                    for n in range(ncolt):
                        if col_filter is not None and not col_filter(g, n):
                            continue
                        for (j0, kp) in pieces:
                            work.append((n, j0, kp))
                    LA = NR - 1
                    for it in range(len(work) + LA):
                        if it < len(work):
                            n, j0, kp = work[it]
                            n0 = n * 512
                            nw = min(512, Ncols - n0)
                            r = it % NR
                            P.dma("pool", wr[r][:, 0:kp, 0:nw], wv[:, j0:j0 + kp, n0:n0 + nw], [], [f"wr{r}"])
                        if it >= LA:
                            n, j0, kp = work[it - LA]
                            n0 = n * 512
                            nw = min(512, Ncols - n0)
                            r = (it - LA) % NR
                            if j0 == 0:
                                par ^= 1
                            for jj in range(kp):
                                j = j0 + jj
                                for ti in range(len(g)):
                                    bk = par * 4 + ti
                                    P.op("pe", (lambda e, r=r, jj=jj, j=j, ti=ti, bk=bk, nw=nw: e.matmul(ps[bk][:, 0:nw], lhsT=aT[0][:, j, ti * 128:(ti + 1) * 128], rhs=wr[r][:, jj, 0:nw], start=(j == 0), stop=(j == KC - 1))),
                                         [f"wr{r}", "aT0"], [f"ps{bk}"])
                            if j0 + kp == KC:
                                for ti in range(len(g)):
                                    epi(state, g[ti], n0, nw, par * 4 + ti)
                P.flush()

        def gated_setup(gidx_of_tile):
            def setup(ph):
                GB = {}
                for gi in sorted(set(gidx_of_tile.values())):
                    GB[gi] = sb(f"GB{gi}", [128, D], F32, ph)
                    P.dma("sp", GB[gi][:], gb_d[gi, :, :], ["gb_d"], [f"GB{gi}"])
                tmp = [sb(f"etmp{i}", [128, 512], F32, ph) for i in range(4)]
                return {"GB": GB, "tmp": tmp, "cnt": [0], "gmap": gidx_of_tile}
            return setup

        def gated_epi(dst_rows):
            def epi(stt, gt, n0, nw, bk):
                k = stt["cnt"][0] % 4
                stt["cnt"][0] += 1
                gi = stt["gmap"][gt]
                G = stt["GB"][gi]
                tmp = stt["tmp"][k]
                P.op("dve", (lambda e: e.tensor_tensor(out=tmp[:, 0:nw], in0=ps[bk][:, 0:nw], in1=G[:, n0:n0 + nw], op=ALU.mult)), [f"ps{bk}", f"GB{gi}"], [f"etmp{k}"])
                r0 = dst_rows(gt)
                P.dma("sp", vd_d[r0:r0 + 128, n0:n0 + nw], tmp[:, 0:nw], [f"etmp{k}"], ["vd_d"])
            return epi

        phase_a_and_ln0()

        nsg = 2 if NT > 17 else 1
        per = (NT + nsg - 1) // nsg
        for sgi in range(nsg):
            t0 = sgi * per
            t1 = min(NT, t0 + per)
            gemm_a(wi[0], t0 * 128, (t1 - t0) * 128)
        allg = [list(range(a, min(a + 4, NT))) for a in range(0, NT, 4)]
        gmap = {gt: (1 if gt < NTC else 0) for gt in range(NT)}
        gemm_tok("ffn1b", hT_d, FC, wo[0], D, allg, gated_epi(lambda gt: gt * 128), gated_setup(gmap))
        tiles = []
        for gt in range(NT):
            r = 1 if gt < NTC else 0
            tiles.append(dict(src_v=vd_d[gt * 128:(gt + 1) * 128, :], src_x=x0_d[gt * 128:(gt + 1) * 128, :], dst_x=x1_d[gt * 128:(gt + 1) * 128, :], dstT=gt * 128, r=r, s=1))
        ln_phase("ln1", tiles, cfg.ALPHA, ln_g[0, :], ln_b[0, :])

        if "stop_ffn1" in taps:
            P.flush()
            return nc

        def win_setup(ph):
            return {"tmp": [sb(f"ptmp{i}", [128, 512], BF16, ph) for i in range(4)],
                    "gt": [sb(f"gtmp{i}", [128, 16], F32, ph) for i in range(2)], "cnt": [0]}

        def win_epi(stt, gt, n0, nw, bk):
            c = stt["cnt"][0]
            stt["cnt"][0] += 1
            if n0 >= PT - 16:
                k = c % 2
                tmp = stt["gt"][k]
                P.op("dve", (lambda e: e.tensor_copy(out=tmp[:, 0:nw], in_=ps[bk][:, 0:nw])), [f"ps{bk}"], [f"gtmp{k}"])
                P.dma("sp", g_d[gt * 128:(gt + 1) * 128, 0:nw], tmp[:, 0:nw], [f"gtmp{k}"], ["g_d"])
            else:
                k = c % 4
                tmp = stt["tmp"][k]
                if c % 2 == 0:
                    P.op("act", (lambda e: e.activation(out=tmp[:, 0:nw], in_=ps[bk][:, 0:nw], func=AF.Copy)), [f"ps{bk}"], [f"ptmp{k}"])
                else:
                    P.op("dve", (lambda e: e.tensor_copy(out=tmp[:, 0:nw], in_=ps[bk][:, 0:nw])), [f"ps{bk}"], [f"ptmp{k}"])
                P.dma("sp", p_d[gt * 128:(gt + 1) * 128, n0:n0 + nw], tmp[:, 0:nw], [f"ptmp{k}"], ["p_d"])

        OWN_END = NTC + NOWN
        n_lo, n_hi = (2 * HYW) // 512, PST0 // 512

        def win_filter(g, n):
            if n_lo <= n < n_hi and all(gt >= OWN_END for gt in g):
                return False
            return True

        gemm_tok("win", aT_d, DC, w_in, PT, allg, win_epi, win_setup, col_filter=win_filter)
        own_cover = set()
        for g in allg:
            if not all(gt >= OWN_END for gt in g):
                own_cover.update(g)
        OWNC_END = max(own_cover) + 1

        def conv_cols(p_col0, ncols, cw, cbias, dst, dst_col0, scale, with_ctx, tile_end=None):
            tile_end = NT if tile_end is None else tile_end
            with contextlib.ExitStack() as ph:
                NB = 2
                Wc = [sb(f"Wc{i}", [128, 3, 512], F32, ph) for i in range(NB)]
                Bc = [sb(f"Bc{i}", [128, 512], F32, ph) for i in range(NB)]
                pt = [sb(f"pt{i}", [128, 512], BF16, ph) for i in range(4)]
                pw = [[sb(f"pw{i}_{k}", [128, 512], BF16, ph) for k in range(3)] for i in range(4)]
                ot = [sb(f"ot{i}", [128, 512], BF16, ph) for i in range(2)]
                cnt = 0
                for cb in range(ncols // 512):
                    b = cb % NB
                    c0 = cb * 512
                    for k in range(3):
                        P.dma("sp", Wc[b][:, k, :], cw[k, c0:c0 + 512].partition_broadcast(128), [], [f"Wc{b}"])
                    P.dma("sp", Bc[b][:], cbias[0, c0:c0 + 512].partition_broadcast(128), [], [f"Bc{b}"])
                    if scale != 1.0:
                        P.op("dve", (lambda e, b=b: e.tensor_scalar(out=Bc[b][:], in0=Bc[b][:], scalar1=scale, scalar2=None, op0=ALU.mult)), [f"Bc{b}"], [f"Bc{b}"])

                    def prods(gt, slot):
                        P.dma("act", pt[slot][:], p_d[gt * 128:(gt + 1) * 128, p_col0 + c0:p_col0 + c0 + 512], ["p_d"], [f"pt{slot}"])
                        for k in range(3):
                            P.op("pool" if k < 2 else "dve", (lambda e, k=k, slot=slot, b=b: e.tensor_tensor(out=pw[slot][k][:], in0=pt[slot][:], in1=Wc[b][:, k, :], op=ALU.mult)),
                                 [f"pt{slot}", f"Wc{b}"], [f"pw{slot}_{k}"])

                    def finish(gt, bk, terms):
                        nonlocal cnt
                        for i, (cidx, slot, k) in enumerate(terms):
                            P.op("pe", (lambda e, cidx=cidx, slot=slot, k=k, i=i, bk=bk, n=len(terms): e.matmul(ps[bk][:], lhsT=cstb[:, cidx, :], rhs=pw[slot][k][:], start=(i == 0), stop=(i == n - 1))),
                                 ["cstb", f"pw{slot}_{k}"], [f"ps{bk}"])
                        o = cnt % 2
                        cnt += 1
                        P.op("dve", (lambda e, o=o, bk=bk, b=b: e.scalar_tensor_tensor(out=ot[o][:], in0=ps[bk][:], scalar=scale, in1=Bc[b][:], op0=ALU.mult, op1=ALU.add)),
                             [f"ps{bk}", f"Bc{b}"], [f"ot{o}"])
                        r0 = gt * 128 if with_ctx else (gt - NTC) * 128
                        P.dma("sp", dst[r0:r0 + 128, dst_col0 + c0:dst_col0 + c0 + 512], ot[o][:], [f"ot{o}"], ["convdst"])

                    if with_ctx:
                        assert NTC == 2
                        prods(0, 0)
                        prods(1, 1)
                        finish(0, 0, [(C_SC0, 0, 0), (C_ID, 0, 1), (C_SC2, 0, 2), (C_EUP, 1, 2)])
                        finish(1, 1, [(C_SC0, 1, 0), (C_ID, 1, 1), (C_SC2, 1, 2), (C_EDN, 0, 0)])
                    for gt in range(NTC, tile_end):
                        slot = gt % 4
                        prods(gt, slot)
                        finish(gt, gt % 4, [(C_SL0, slot, 0), (C_ID, slot, 1), (C_SL2, slot, 2)])
                P.flush()

        conv_cols(0, 2 * HYW, hy_cw, hy_cb, u3_d, 0, 1.0, False)
        conv_cols(2 * HYW, HYW, hy_cw[:, 2 * HYW:PHY], hy_cb[:, 2 * HYW:PHY], u3_d, 2 * HYW, 1.0, False, tile_end=OWN_END)
        conv_cols(PHY, MLQK, ml_cw[:, 0:MLQK], ml_cb[:, 0:MLQK], q_d, 0, float(DQK) ** -0.5, False, tile_end=OWN_END)
        conv_cols(PST0, MLQK, ml_cw[:, MLQK:2 * MLQK], ml_cb[:, MLQK:2 * MLQK], k_d, 0, 1.0, True)

        if "stop_conv" in taps:
            P.flush()
            return nc

        CB, NCB, NF, NH = cfg.CB, cfg.NCB, cfg.NF, cfg.NF // 2
        FH = cfg.FH
        PI = math.pi
        gfa_d = dscr("gfa_d", [NCB, 2, NF, 128, CB], F32)

        with contextlib.ExitStack() as ph:
            zT = sb("zT", [cfg.EMB, L], F32, ph)
            w1 = sb("w1", [cfg.EMB, FH], F32, ph)
            w2 = sb("w2", [FH, FH], F32, ph)
            w3 = sb("w3", [FH, 4 * HYW], F32, ph)
            fv = sb("fv", [FH, 6], F32, ph)
            h1T = sb("h1T", [FH, L], F32, ph)
            h2T = sb("h2T", [FH, L], F32, ph)
            negt = sb("negt", [128, NTL], F32, ph)
            dlt = sb("dlt", [128, HYW], F32, ph)
            P.dma("sp", zT[:], zT_d[:, :], [], ["zT"])
            P.dma("sp", w1[:], fw1[:, :], [], ["w1"])
            P.dma("sp", w2[:], fw2[:, :], [], ["w2"])
            P.dma("act", w3[:], fw3[:, :], [], ["w3"])
            for i, a in enumerate([fb1, ff1, fb2, ff2]):
                P.dma("sp", fv[:, i:i + 1], a[:, :], [], ["fv"])
            P.dma("sp", negt[:], negt_d[:, :], [], ["negt"])
            P.dma("sp", dlt[:], deltas_d[0, :].partition_broadcast(128), [], ["dlt"])
            P.op("dve", lambda e: e.tensor_tensor(out=fv[:, 4:5], in0=fv[:, 0:1], in1=fv[:, 1:2], op=ALU.mult), ["fv"], ["fv"])
            P.op("dve", lambda e: e.tensor_tensor(out=fv[:, 5:6], in0=fv[:, 2:3], in1=fv[:, 3:4], op=ALU.mult), ["fv"], ["fv"])
            atmp = [sb(f"atmp{i}", [FH, 512], F32, ph) for i in range(2)]
            s4t = [sb(f"s4t{i}", [FH, 512], F32, ph) for i in range(2)]
            s8t = [sb(f"s8t{i}", [FH, 512], F32, ph) for i in range(2)]

            def sin_layer(wt, K, src, dst, fcol, fbcol):
                dres = "h1T" if dst is h1T else "h2T"
                for bi, t0 in enumerate(range(0, L, 512)):
                    k = bi % 2
                    bk = bi % 2
                    A, S4, S8 = atmp[k], s4t[k], s8t[k]
                    ra, r4, r8 = f"atmp{k}", f"s4t{k}", f"s8t{k}"
                    P.op("pe", (lambda e, t0=t0, bk=bk: e.matmul(ps[bk][0:FH, :], lhsT=wt[0:K, :], rhs=src[0:K, t0:t0 + 512], start=True, stop=True)), ["w1", "w2", "zT", "h1T"], [f"ps{bk}"])
                    P.op("act", (lambda e, A=A, bk=bk: e.activation(out=A[:], in_=ps[bk][0:FH, :], func=AF.Identity, bias=fv[:, fbcol:fbcol + 1], scale=fv[:, fcol:fcol + 1])), [f"ps{bk}", "fv"], [ra])
                    P.op("act", (lambda e, A=A, S4=S4: e.activation(out=S4[:], in_=A[:], func=AF.Sin, scale=0.25)), [ra], [r4])
                    P.op("act", (lambda e, A=A, S8=S8: e.activation(out=S8[:], in_=A[:], func=AF.Sin, scale=0.125)), [ra], [r8])
                    P.op("dve", (lambda e, S8=S8: e.tensor_tensor(out=S8[:], in0=S8[:], in1=S8[:], op=ALU.mult)), [r8], [r8])
                    P.op("dve", (lambda e, S8=S8: e.tensor_scalar(out=S8[:], in0=S8[:], scalar1=-2.0, scalar2=1.0, op0=ALU.mult, op1=ALU.add)), [r8], [r8])
                    P.op("dve", (lambda e, S8=S8, S4=S4: e.scalar_tensor_tensor(out=S8[:], in0=S4[:], scalar=2.0, in1=S8[:], op0=ALU.mult, op1=ALU.mult)), [r4, r8], [r8])
                    P.op("dve", (lambda e, S4=S4: e.tensor_tensor(out=S4[:], in0=S4[:], in1=S4[:], op=ALU.mult)), [r4], [r4])
                    P.op("dve", (lambda e, S4=S4: e.tensor_scalar(out=S4[:], in0=S4[:], scalar1=-2.0, scalar2=1.0, op0=ALU.mult, op1=ALU.add)), [r4], [r4])
                    P.op("dve", (lambda e, S4=S4, S8=S8, t0=t0: e.scalar_tensor_tensor(out=dst[:, t0:t0 + 512], in0=S8[:], scalar=2.0, in1=S4[:], op0=ALU.mult, op1=ALU.mult)), [r4, r8], [dres])

            sin_layer(w1, cfg.EMB, zT, h1T, 1, 4)
            sin_layer(w2, FH, h1T, h2T, 3, 5)
            win = sb("win", [128, NTL, 512], F32, ph)
            hwt = [sb(f"hwt{i}", [128, 512], F32, ph) for i in range(4)]
            hab = [sb(f"hab{i}", [128, 512], F32, ph) for i in range(2)]
            rn = sb("rn", [128, 512], F32, ph)
            lf = [sb(f"lf{i}", [128, 2, 512], F32, ph) for i in range(2)]
            hso = [sb(f"hso{i}", [128, 2, 512], BF16, ph) for i in range(2)]
            cnt = 0
            for cbk in range(HYW // 512):
                c0 = cbk * 512
                for tt in range(NTL):
                    P.op("act", (lambda e, tt=tt, c0=c0: e.activation(out=win[:, tt, :], in_=dlt[:, c0:c0 + 512], func=AF.Exp, scale=negt[:, tt:tt + 1])), ["negt", "dlt"], ["win"])
                for o in range(2):
                    for tt in range(NTL):
                        for dr in range(2):
                            col = o * 2 * HYW + dr * HYW + c0
                            bk = 2 + cnt % 2
                            k = cnt % 4
                            ka = cnt % 2
                            cnt += 1
                            P.op("pe", (lambda e, tt=tt, col=col, bk=bk: e.matmul(ps[bk][:], lhsT=h2T[:, tt * 128:(tt + 1) * 128], rhs=w3[:, col:col + 512], start=True, stop=True)), ["h2T", "w3"], [f"ps{bk}"])
                            P.op("dve", (lambda e, tt=tt, bk=bk, k=k: e.tensor_tensor(out=hwt[k][:], in0=ps[bk][:], in1=win[:, tt, :], op=ALU.mult)), [f"ps{bk}", "win"], [f"hwt{k}"])
                            P.dma("sp", hw_d[tt * 128:(tt + 1) * 128, col:col + 512], hwt[k][:], [f"hwt{k}"], ["hw_d"])
                            P.op("act", (lambda e, k=k, ka=ka: e.activation(out=hab[ka][:], in_=hwt[k][:], func=AF.Abs)), [f"hwt{k}"], [f"hab{ka}"])
                            first = (tt == 0 and dr == 0)
                            last = (tt == NTL - 1 and dr == 1)
                            P.op("pe", (lambda e, ka=ka, first=first, last=last: e.matmul(ps[4][:], lhsT=ones, rhs=hab[ka][:], start=first, stop=last)), [f"hab{ka}", "cst"], ["ps4"])
                    P.op("dve", lambda e: e.reciprocal(out=rn[:], in_=ps[4][:]), ["ps4"], ["rn"])
                    for tt in range(NTL):
                        k = tt % 2
                        for dr in range(2):
                            col = o * 2 * HYW + dr * HYW + c0
                            P.dma("act", lf[k][:, dr, :], hw_d[tt * 128:(tt + 1) * 128, col:col + 512], ["hw_d"], [f"lf{k}"])
                        P.op("pool", (lambda e, k=k: e.tensor_tensor(out=hab[0][:], in0=lf[k][:, 0, :], in1=lf[k][:, 1, :], op=ALU.add)), [f"lf{k}"], ["hab0"])
                        P.op("pool", (lambda e, k=k: e.tensor_tensor(out=hab[1][:], in0=lf[k][:, 0, :], in1=lf[k][:, 1, :], op=ALU.subtract)), [f"lf{k}"], ["hab1"])
                        P.op("dve", (lambda e, k=k: e.tensor_tensor(out=hso[k][:, 0, :], in0=hab[0][:], in1=rn[:], op=ALU.mult)), ["hab0", "rn"], [f"hso{k}"])
                        P.op("dve", (lambda e, k=k: e.tensor_tensor(out=hso[k][:, 1, :], in0=hab[1][:], in1=rn[:], op=ALU.mult)), ["hab1", "rn"], [f"hso{k}"])
                        for sd in range(2):
                            P.dma("sp", hsd_d[o, sd, tt * 128:(tt + 1) * 128, c0:c0 + 512], hso[k][:, sd, :], [f"hso{k}"], ["hsd_d"])
            P.flush()

        if "stop_filt" in taps:
            return nc

        def load_tok_tiles(dst, src2d, col0, res):
            v = src2d.rearrange("(tt p) c -> p tt c", p=128)
            q4 = max(1, NTL // 4)
            for a in range(0, NTL, q4):
                P.dma("act", dst[:, a:a + q4, :], v[:, a:a + q4, col0:col0 + CB], ["hsd_d", "u3_d", "z_d"], [res])

        NRF = 4
        for cbk in range(NCB):
            c0 = cbk * CB
            for o in range(2):
                with contextlib.ExitStack() as ph:
                    Hs = sb("Hs", [128, NTL, CB], BF16, ph)
                    Hd = sb("Hd", [128, NTL, CB], BF16, ph)
                    fwb = [sb(f"fwb{i}", [128, NTL, 128], BF16, ph) for i in range(NRF)]
                    go = [sb(f"go{i}", [128, CB], F32, ph) for i in range(2)]
                    load_tok_tiles(Hs, hsd_d[o, 0], c0, "Hs")
                    load_tok_tiles(Hd, hsd_d[o, 1], c0, "Hd")
                    LA = NRF - 1
                    for it in range(NF + LA):
                        if it < NF:
                            r = it % NRF
                            P.dma("sp", fwb[r][:], fwr_d[it, :, :, :], [], [f"fwb{r}"])
                        if it >= LA:
                            fc = it - LA
                            r = fc % NRF
                            bk = fc % 2
                            src, rs = (Hs, "Hs") if fc < NH else (Hd, "Hd")
                            for tt in range(NTL):
                                P.op("pe", (lambda e, r=r, tt=tt, bk=bk, src=src: e.matmul(ps[bk][:, 0:CB], lhsT=fwb[r][:, tt, :], rhs=src[:, tt, :], start=(tt == 0), stop=(tt == NTL - 1))), [f"fwb{r}", rs], [f"ps{bk}"])
                            if fc == NH:
                                for tt in range(NTL):
                                    P.op("pe", (lambda e, r=r, tt=tt: e.matmul(ps[2][0:1, 0:CB], lhsT=fwb[r][:, tt, 0:1], rhs=Hs[:, tt, :], start=(tt == 0), stop=(tt == NTL - 1))), [f"fwb{r}", "Hs"], ["ps2"])
                            k = fc % 2
                            P.op("act", (lambda e, k=k, bk=bk: e.activation(out=go[k][:], in_=ps[bk][:, 0:CB], func=AF.Copy)), [f"ps{bk}"], [f"go{k}"])
                            if fc == NH:
                                P.op("dve", (lambda e, k=k: e.tensor_copy(out=go[k][0:1, :], in_=ps[2][0:1, 0:CB])), ["ps2", f"go{k}"], [f"go{k}"])
                            P.dma("sp", gfa_d[cbk, o, fc, :, :], go[k][:], [f"go{k}"], ["gfa_d"])
                    P.flush()

        NRI = 2
        hyb = hy_bias
        for cbk in range(NCB):
            c0 = cbk * CB
            for o in range(2):
                with contextlib.ExitStack() as ph:
                    dat = sb("dat", [128, NTL, CB], BF16, ph)
                    Ys = sb("Ys", [128, NF, CB], BF16, ph)
                    if o == 0:
                        load_tok_tiles(dat, u3_d, c0, "dat")
                    else:
                        load_tok_tiles(dat, z_d, c0, "dat")
                    with contextlib.ExitStack() as ph2:
                        fwb = [sb(f"fwc{i}", [128, NTL, 128], BF16, ph2) for i in range(NRF)]
                        gt_ = [sb(f"gt{i}", [128, 2, CB], F32, ph2) for i in range(2)]
                        tq = [[sb(f"tq{i}_{j}", [128, CB], F32, ph2) for j in range(4)] for i in range(2)]
                        order = []
                        for fc in range(NH):
                            order += [fc, NH + fc]
                        LA = NRF - 1
                        for it in range(NF + LA):
                            if it < NF:
                                r = it % NRF
                                P.dma("sp", fwb[r][:], fwr_d[order[it], :, :, :], [], [f"fwc{r}"])
                            if it >= LA:
                                idx = it - LA
                                fchunk = order[idx]
                                r = idx % NRF
                                fc = fchunk % NH
                                isb = fchunk >= NH
                                bk = (fc % 2) * 2 + (1 if isb else 0)
                                for tt in range(NTL):
                                    P.op("pe", (lambda e, r=r, tt=tt, bk=bk: e.matmul(ps[bk][:, 0:CB], lhsT=fwb[r][:, tt, :], rhs=dat[:, tt, :], start=(tt == 0), stop=(tt == NTL - 1))), [f"fwc{r}", "dat"], [f"ps{bk}"])
                                if isb:
                                    k = fc % 2
                                    pa, pb = (fc % 2) * 2, (fc % 2) * 2 + 1
                                    P.dma("act", gt_[k][:, 0, :], gfa_d[cbk, o, fc, :, :], ["gfa_d"], [f"gt{k}"])
                                    P.dma("act", gt_[k][:, 1, :], gfa_d[cbk, o, NH + fc, :, :], ["gfa_d"], [f"gt{k}"])
                                    T1, T2, T3, T4 = tq[k]
                                    rq = [f"tq{k}_{j}" for j in range(4)]
                                    P.op("dve", (lambda e, k=k, pa=pa, T1=T1: e.tensor_tensor(out=T1[:], in0=ps[pa][:, 0:CB], in1=gt_[k][:, 0, :], op=ALU.mult)), [f"ps{pa}", f"gt{k}"], [rq[0]])
                                    P.op("dve", (lambda e, k=k, pb=pb, T2=T2: e.tensor_tensor(out=T2[:], in0=ps[pb][:, 0:CB], in1=gt_[k][:, 1, :], op=ALU.mult)), [f"ps{pb}", f"gt{k}"], [rq[1]])
                                    P.op("dve", (lambda e, k=k, pa=pa, T3=T3: e.tensor_tensor(out=T3[:], in0=ps[pa][:, 0:CB], in1=gt_[k][:, 1, :], op=ALU.mult)), [f"ps{pa}", f"gt{k}"], [rq[2]])
                                    P.op("dve", (lambda e, k=k, pb=pb, T4=T4: e.tensor_tensor(out=T4[:], in0=ps[pb][:, 0:CB], in1=gt_[k][:, 0, :], op=ALU.mult)), [f"ps{pb}", f"gt{k}"], [rq[3]])
                                    P.op("pool", (lambda e, fc=fc, T1=T1, T2=T2: e.tensor_tensor(out=Ys[:, fc, :], in0=T1[:], in1=T2[:], op=ALU.subtract)), [rq[0], rq[1]], ["Ys"])
                                    P.op("pool", (lambda e, fc=fc, T3=T3, T4=T4: e.tensor_tensor(out=Ys[:, NH + fc, :], in0=T3[:], in1=T4[:], op=ALU.add)), [rq[2], rq[3]], ["Ys"])
                                    if fc == 0:
                                        P.op("pool", (lambda e, T1=T1: e.tensor_copy(out=Ys[0:1, 0, :], in_=T1[0:1, :])), [rq[0], "Ys"], ["Ys"])
                                        P.op("pool", (lambda e, T2=T2: e.tensor_copy(out=Ys[0:1, NH, :], in_=T2[0:1, :])), [rq[1], "Ys"], ["Ys"])
                        P.flush()
                    with contextlib.ExitStack() as ph2:
                        ivb = [sb(f"ivb{i}", [128, NF, 128], BF16, ph2) for i in range(NRI)]
                        bt = sb("bt", [128, CB], F32, ph2)
                        xt_ = [sb(f"xt{i}", [128, CB], BF16, ph2) for i in range(2)]
                        e1 = [sb(f"e1{i}", [128, CB], F32, ph2) for i in range(2)]
                        zo = [sb(f"zo{i}", [128, CB], BF16, ph2) for i in range(2)]
                        yo = [sb(f"yo{i}", [128, CB], F32, ph2) for i in range(2)]
                        ysb = [sb(f"ysb{i}", [128, CB // 128, 128], BF16, ph2) for i in range(2)]
                        P.dma("sp", bt[:], hyb[o, c0:c0 + CB].partition_broadcast(128), [], ["bt"])
                        xcol = (1 + o) * HYW + c0
                        mixv = mixT_d.rearrange("c p t -> p c t")
                        NTI = NTL if o == 0 else NOWN
                        for it in range(NTI + 1):
                            if it < NTI:
                                r = it % NRI
                                P.dma("sp", ivb[r][:], ivr_d[it, :, :, :], [], [f"ivb{r}"])
                            if it >= 1:
                                tc_ = it - 1
                                r = tc_ % NRI
                                bk = tc_ % 2
                                k = tc_ % 2
                                P.dma("act", xt_[k][:], u3_d[tc_ * 128:(tc_ + 1) * 128, xcol:xcol + CB], ["u3_d"], [f"xt{k}"])
                                for fq in range(NF):
                                    P.op("pe", (lambda e, r=r, fq=fq, bk=bk: e.matmul(ps[bk][:, 0:CB], lhsT=ivb[r][:, fq, :], rhs=Ys[:, fq, :], start=(fq == 0), stop=(fq == NF - 1))), [f"ivb{r}", "Ys"], [f"ps{bk}"])
                                P.op("pool", (lambda e, k=k, tc_=tc_: e.tensor_tensor(out=e1[k][:], in0=dat[:, tc_, :], in1=bt[:], op=ALU.mult)), ["dat", "bt"], [f"e1{k}"])
                                P.op("dve", (lambda e, k=k, bk=bk: e.tensor_tensor(out=e1[k][:], in0=e1[k][:], in1=ps[bk][:, 0:CB], op=ALU.add)), [f"e1{k}", f"ps{bk}"], [f"e1{k}"])
                                if o == 0:
                                    P.op("pool", (lambda e, k=k: e.tensor_tensor(out=zo[k][:], in0=e1[k][:], in1=xt_[k][:], op=ALU.mult)), [f"e1{k}", f"xt{k}"], [f"zo{k}"])
                                    P.dma("sp", z_d[tc_ * 128:(tc_ + 1) * 128, c0:c0 + CB], zo[k][:], [f"zo{k}"], ["z_d"])
                                else:
                                    P.op("pool", (lambda e, k=k: e.tensor_tensor(out=yo[k][:], in0=e1[k][:], in1=xt_[k][:], op=ALU.mult)), [f"e1{k}", f"xt{k}"], [f"yo{k}"])
                                    pb2 = 4 + tc_ % 2
                                    for i4 in range(CB // 128):
                                        P.op("pe", (lambda e, k=k, i4=i4, pb2=pb2: e.transpose(out=ps[pb2][:, i4 * 128:(i4 + 1) * 128], in_=yo[k][:, i4 * 128:(i4 + 1) * 128], identity=ident)), [f"yo{k}", "cst"], [f"ps{pb2}"])
                                    P.op("act", (lambda e, k=k, pb2=pb2: e.activation(out=ysb[k][:].rearrange("p c t -> p (c t)"), in_=ps[pb2][:, 0:CB], func=AF.Copy)), [f"ps{pb2}"], [f"ysb{k}"])
                                    cc0 = c0 // 128
                                    P.dma("sp", mixv[:, cc0:cc0 + CB // 128, tc_ * 128:(tc_ + 1) * 128], ysb[k][:], [f"ysb{k}"], ["mixT_d"])
                        P.flush()

        if "stop_hy" in taps:
            return nc

        VC0 = PST0 + MLQK
        OC0 = PHY + MLQK
        with contextlib.ExitStack() as ph:
            AA = sb("AA", [128, NT, 8], F32, ph)
            EBB = sb("EBB", [128, NT, 8], F32, ph)
            EE = sb("EE", [128, NT, 8], F32, ph)
            gbb = sb("gbb", [128, 16], F32, ph)
            one_t = sb("one_t", [128, 1], F32, ph)
            P.dma("sp", gbb[:], ml_gb[0, :].partition_broadcast(128), [], ["gbb"])
            P.op("dve", lambda e: e.memset(one_t[:], 1.0), [], ["one_t"])
            gtl = [sb(f"gtl{i}", [128, 16], F32, ph) for i in range(2)]
            lp = [sb(f"lp{i}", [128, 8], F32, ph) for i in range(2)]
            igt = [sb(f"igt{i}", [128, 8], F32, ph) for i in range(2)]
            for c in range(NT):
                k = c % 2
                bk = c % 2
                G_, LP, IG = gtl[k], lp[k], igt[k]
                P.dma("sp", G_[:], g_d[c * 128:(c + 1) * 128, :], ["g_d"], [f"gtl{k}"])
                P.op("dve", (lambda e, G_=G_: e.tensor_tensor(out=G_[:], in0=G_[:], in1=gbb[:], op=ALU.add)), [f"gtl{k}", "gbb"], [f"gtl{k}"])
                for dr in range(2):
                    P.op("act", (lambda e, G_=G_, LP=LP, dr=dr: e.activation(out=LP[:, dr * 4:dr * 4 + 4], in_=G_[:, dr * 8 + 4:dr * 8 + 8], func=AF.Exp, scale=-1.0)), [f"gtl{k}"], [f"lp{k}"])
                    P.op("dve", (lambda e, G_=G_, IG=IG, dr=dr: e.tensor_copy(out=IG[:, dr * 4:dr * 4 + 4], in_=G_[:, dr * 8:dr * 8 + 4])), [f"gtl{k}"], [f"igt{k}"])
                P.op("act", (lambda e, LP=LP: e.activation(out=LP[:], in_=LP[:], func=AF.Ln, bias=one_t[:, 0:1], scale=1.0)), [f"lp{k}", "one_t"], [f"lp{k}"])
                P.op("pe", (lambda e, LP=LP, bk=bk: e.matmul(ps[bk][:, 0:4], lhsT=cst[:, C_MF, :], rhs=LP[:, 0:4], start=True, stop=True)), [f"lp{k}", "cst"], [f"ps{bk}"])
                P.op("pe", (lambda e, LP=LP, bk=bk: e.matmul(ps[bk][:, 4:8], lhsT=cst[:, C_MB, :], rhs=LP[:, 4:8], start=True, stop=True)), [f"lp{k}", "cst"], [f"ps{bk}"])
                P.op("pe", (lambda e, LP=LP, bk=bk: e.matmul(ps[bk][:, 8:16], lhsT=ones, rhs=LP[:, 0:8], start=True, stop=True)), [f"lp{k}", "cst"], [f"ps{bk}"])
                P.op("dve", (lambda e, IG=IG, bk=bk: e.tensor_tensor(out=IG[:], in0=IG[:], in1=ps[bk][:, 0:8], op=ALU.add)), [f"igt{k}", f"ps{bk}"], [f"igt{k}"])
                P.op("act", (lambda e, IG=IG, c=c: e.activation(out=AA[:, c, :], in_=IG[:], func=AF.Exp)), [f"igt{k}"], ["AA"])
                P.op("act", (lambda e, c=c, bk=bk: e.activation(out=EBB[:, c, :], in_=ps[bk][:, 0:8], func=AF.Exp)), [f"ps{bk}"], ["EBB"])
                P.op("act", (lambda e, c=c, bk=bk: e.activation(out=EE[:, c, :], in_=ps[bk][:, 8:16], func=AF.Exp, scale=-1.0)), [f"ps{bk}"], ["EE"])

            Chat = [[sb(f"Chat{d}_{h}", [128, NDC, DV], F32, ph) for h in range(H)] for d in range(2)]
            nhat = [[sb(f"nhat{d}_{h}", [128, NDC], F32, ph) for h in range(H)] for d in range(2)]
            Cb = [[sb(f"Cb{d}_{h}", [128, NDC, DV], BF16, ph) for h in range(H)] for d in range(2)]
            nb = [[sb(f"nb{d}_{h}", [128, NDC], BF16, ph) for h in range(H)] for d in range(2)]
            for d in range(2):
                for h in range(H):
                    P.op("pool", (lambda e, d=d, h=h: e.memset(Chat[d][h][:], 0.0)), [], [f"Chat{d}_{h}"])
                    P.op("pool", (lambda e, d=d, h=h: e.memset(nhat[d][h][:], 0.0)), [], [f"nhat{d}_{h}"])
                    P.op("pool", (lambda e, d=d, h=h: e.memset(Cb[d][h][:], 0.0)), [], [f"Cb{d}_{h}"])
                    P.op("pool", (lambda e, d=d, h=h: e.memset(nb[d][h][:], 0.0)), [], [f"nb{d}_{h}"])
            kt = [sb(f"kt{d}", [128, MLQK], BF16, ph) for d in range(2)]
            qt = [sb(f"qt{d}", [128, MLQK], BF16, ph) for d in range(2)]
            vt = [sb(f"vt{d}", [128, MLW], BF16, ph) for d in range(2)]
            kT = [sb(f"kT{d}", [128, H * NDC, 128], BF16, ph) for d in range(2)]
            qT = [sb(f"qT{d}", [128, H * NDC, 128], BF16, ph) for d in range(2)]
            VA = [sb(f"VA{i}", [128, DV], BF16, ph) for i in range(2)]
            acol = [sb(f"acol{i}", [128, 1], BF16, ph) for i in range(2)]
            STb = [sb(f"STb{i}", [128, 128], BF16, ph) for i in range(2)]
            rr = [sb(f"rr{i}", [128, 2], F32, ph) for i in range(2)]
            ho = [sb(f"ho{i}", [128, DV], F32, ph) for i in range(2)]
            order0 = list(range(NT))
            order1 = list(range(NTC - 1, -1, -1)) + list(range(NT - 1, NTC - 1, -1))
            orders = [order0, order1]
            psb = [ps[0][:].bitcast(BF16), ps[1][:].bitcast(BF16)]
            ucnt = 0
            NTR = H * NDC
            for step in range(NT):
                for d in range(2):
                    c = orders[d][step]
                    cprev = orders[d][step - 1] if step > 0 else c
                    if d == 0 and c >= OWN_END:
                        continue
                    lat = NTC <= c < OWN_END
                    P.dma("sp", kt[d][:], k_d[c * 128:(c + 1) * 128, :], ["k_d"], [f"kt{d}"])
                    P.dma("act", vt[d][:], p_d[c * 128:(c + 1) * 128, VC0:VC0 + MLW], ["p_d"], [f"vt{d}"])
                    if lat:
                        P.dma("sp", qt[d][:], q_d[(c - NTC) * 128:(c - NTC + 1) * 128, :], ["q_d"], [f"qt{d}"])
                        for (src, dstT, rs, rd) in ((kt[d], kT[d], f"kt{d}", f"kT{d}"), (qt[d], qT[d], f"qt{d}", f"qT{d}")):
                            for i0 in range(0, NTR, 8):
                                nn = min(8, NTR - i0)
                                bkT = (i0 // 8) % 2
                                for i in range(nn):
                                    P.op("pe", (lambda e, src=src, i=i, i0=i0, bkT=bkT: e.transpose(out=psb[bkT][:, i * 128:(i + 1) * 128], in_=src[:, (i0 + i) * 128:(i0 + i + 1) * 128], identity=cstb[:, C_ID, :])), [rs, "cstb"], [f"ps{bkT}"])
                                P.op("act", (lambda e, dstT=dstT, i0=i0, nn=nn, bkT=bkT: e.activation(out=dstT[:, i0:i0 + nn, :].rearrange("p c t -> p (c t)"), in_=psb[bkT][:, 0:nn * 128], func=AF.Copy)), [f"ps{bkT}"], [rd])
                    for h in range(H):
                        u = ucnt % 2
                        ucnt += 1
                        gcol = d * 4 + h
                        rC, rn_, rCb, rnb = f"Chat{d}_{h}", f"nhat{d}_{h}", f"Cb{d}_{h}", f"nb{d}_{h}"
                        P.op("dve", (lambda e, u=u, d=d, h=h, c=c, gcol=gcol: e.tensor_scalar(out=VA[u][:], in0=vt[d][:, h * DV:(h + 1) * DV], scalar1=AA[:, c, gcol:gcol + 1], scalar2=None, op0=ALU.mult)), [f"vt{d}", "AA"], [f"VA{u}"])
                        P.op("dve", (lambda e, u=u, c=c, gcol=gcol: e.tensor_copy(out=acol[u][:], in_=AA[:, c, gcol:gcol + 1])), ["AA"], [f"acol{u}"])
                        if lat:
                            bS = 2 + u
                            for dc in range(NDC):
                                P.op("pe", (lambda e, d=d, h=h, dc=dc, bS=bS: e.matmul(ps[bS][:, 0:128], lhsT=kT[d][:, h * NDC + dc, :], rhs=qT[d][:, h * NDC + dc, :], start=(dc == 0), stop=(dc == NDC - 1))), [f"kT{d}", f"qT{d}"], [f"ps{bS}"])
                            mk = C_MF if d == 0 else C_MB
                            P.op("dve", (lambda e, u=u, bS=bS, mk=mk: e.tensor_tensor(out=STb[u][:], in0=ps[bS][:, 0:128], in1=cst[:, mk, :], op=ALU.mult)), [f"ps{bS}", "cst"], [f"STb{u}"])
                            bN = 4 + u
                            P.op("pe", (lambda e, u=u, bN=bN: e.matmul(ps[bN][:, 0:DV], lhsT=STb[u][:], rhs=VA[u][:], start=True, stop=False)), [f"STb{u}", f"VA{u}"], [f"ps{bN}"])
                            for dc in range(NDC):
                                P.op("pe", (lambda e, d=d, h=h, dc=dc, bN=bN: e.matmul(ps[bN][:, 0:DV], lhsT=qT[d][:, h * NDC + dc, :], rhs=Cb[d][h][:, dc, :], start=False, stop=(dc == NDC - 1))), [f"qT{d}", rCb], [f"ps{bN}"])
                            P.op("pe", (lambda e, u=u, bS=bS: e.matmul(ps[bS][:, 256:257], lhsT=STb[u][:], rhs=acol[u][:], start=True, stop=False)), [f"STb{u}", f"acol{u}"], [f"ps{bS}"])
                            for dc in range(NDC):
                                P.op("pe", (lambda e, d=d, h=h, dc=dc, bS=bS: e.matmul(ps[bS][:, 256:257], lhsT=qT[d][:, h * NDC + dc, :], rhs=nb[d][h][:, dc:dc + 1], start=False, stop=(dc == NDC - 1))), [f"qT{d}", rnb], [f"ps{bS}"])
                            P.op("act", (lambda e, u=u, bS=bS: e.activation(out=rr[u][:, 0:1], in_=ps[bS][:, 256:257], func=AF.Abs)), [f"ps{bS}"], [f"rr{u}"])
                            P.op("dve", (lambda e, u=u, c=c, gcol=gcol: e.tensor_tensor(out=rr[u][:, 0:1], in0=rr[u][:, 0:1], in1=EBB[:, c, gcol:gcol + 1], op=ALU.max)), [f"rr{u}", "EBB"], [f"rr{u}"])
                            P.op("dve", (lambda e, u=u: e.reciprocal(out=rr[u][:, 1:2], in_=rr[u][:, 0:1])), [f"rr{u}"], [f"rr{u}"])
                            P.op("act", (lambda e, u=u, bN=bN: e.activation(out=ho[u][:], in_=ps[bN][:, 0:DV], func=AF.Copy, scale=rr[u][:, 1:2])), [f"ps{bN}", f"rr{u}"], [f"ho{u}"])
                            P.dma("sp", hf_d[d, (c - NTC) * 128:(c - NTC + 1) * 128, h * DV:(h + 1) * DV], ho[u][:], [f"ho{u}"], ["hf_d"])
                        for dc in range(NDC):
                            bU = 6 + dc % 2
                            P.op("pe", (lambda e, d=d, h=h, dc=dc, u=u, bU=bU: e.matmul(ps[bU][:, 0:DV], lhsT=kt[d][:, h * DQK + dc * 128:h * DQK + (dc + 1) * 128], rhs=VA[u][:], start=True, stop=True)), [f"kt{d}", f"VA{u}"], [f"ps{bU}"])
                            P.op("pe", (lambda e, d=d, h=h, dc=dc, u=u, bU=bU: e.matmul(ps[bU][:, DV:DV + 1] if DV < 512 else ps[2 + u][:, 300 + dc:301 + dc], lhsT=kt[d][:, h * DQK + dc * 128:h * DQK + (dc + 1) * 128], rhs=acol[u][:], start=True, stop=True)), [f"kt{d}", f"acol{u}"], [f"ps{bU}", f"ps{2 + u}"])
                            nps = (lambda dc=dc, bU=bU, u=u: ps[bU][:, DV:DV + 1] if DV < 512 else ps[2 + u][:, 300 + dc:301 + dc])
                            P.op("dve", (lambda e, d=d, h=h, dc=dc, bU=bU, cprev=cprev, gcol=gcol: e.scalar_tensor_tensor(out=Chat[d][h][:, dc, :], in0=Chat[d][h][:, dc, :], scalar=EE[:, cprev, gcol:gcol + 1], in1=ps[bU][:, 0:DV], op0=ALU.mult, op1=ALU.add)), [rC, "EE", f"ps{bU}"], [rC])
                            P.op("dve", (lambda e, d=d, h=h, dc=dc, cprev=cprev, gcol=gcol, nps=nps: e.scalar_tensor_tensor(out=nhat[d][h][:, dc:dc + 1], in0=nhat[d][h][:, dc:dc + 1], scalar=EE[:, cprev, gcol:gcol + 1], in1=nps(), op0=ALU.mult, op1=ALU.add)), [rn_, "EE", f"ps{bU}", f"ps{2 + u}"], [rn_])
                            P.op("act", (lambda e, d=d, h=h, dc=dc, c=c, gcol=gcol: e.activation(out=Cb[d][h][:, dc, :], in_=Chat[d][h][:, dc, :], func=AF.Copy, scale=EE[:, c, gcol:gcol + 1])), [rC, "EE"], [rCb])
                            P.op("act", (lambda e, d=d, h=h, dc=dc, c=c, gcol=gcol: e.activation(out=nb[d][h][:, dc:dc + 1], in_=nhat[d][h][:, dc:dc + 1], func=AF.Copy, scale=EE[:, c, gcol:gcol + 1])), [rn_, "EE"], [rnb])
            P.flush()

        with contextlib.ExitStack() as ph:
            NWb = sb("NWb", [128, MLW], F32, ph)
            P.dma("sp", NWb[:], ml_nw[0, :].partition_broadcast(128), [], ["NWb"])
            hA = [sb(f"hA{i}", [128, MLW], F32, ph) for i in range(2)]
            hB = [sb(f"hB{i}", [128, MLW], F32, ph) for i in range(2)]
            ob = [sb(f"ob{i}", [128, MLW], BF16, ph) for i in range(2)]
            og = [sb(f"og{i}", [128, MLW], F32, ph) for i in range(2)]
            st2 = [sb(f"st2{i}", [128, H, 6], F32, ph) for i in range(2)]
            mv2 = [sb(f"mv2{i}", [128, H, 4], F32, ph) for i in range(2)]
            msb = [sb(f"msb{i}", [128, MLW // 128, 128], BF16, ph) for i in range(2)]
            mixv = mixT_d.rearrange("c p t -> p c t")
            for tl_ in range(NOWN):
                k = tl_ % 2
                A_, B_, O_, OG, S2, M2, MS = hA[k], hB[k], ob[k], og[k], st2[k], mv2[k], msb[k]
                P.dma("sp", A_[:], hf_d[0, tl_ * 128:(tl_ + 1) * 128, :], ["hf_d"], [f"hA{k}"])
                P.dma("act", B_[:], hf_d[1, tl_ * 128:(tl_ + 1) * 128, :], ["hf_d"], [f"hB{k}"])
                P.dma("sp", O_[:], p_d[(NTC + tl_) * 128:(NTC + tl_ + 1) * 128, OC0:OC0 + MLW], ["p_d"], [f"ob{k}"])
                P.op("pool", (lambda e, A_=A_, B_=B_: e.tensor_tensor(out=A_[:], in0=A_[:], in1=B_[:], op=ALU.add)), [f"hA{k}", f"hB{k}"], [f"hA{k}"])
                P.op("act", (lambda e, O_=O_, OG=OG: e.activation(out=OG[:], in_=O_[:], func=AF.Sigmoid)), [f"ob{k}"], [f"og{k}"])
                P.op("pool", (lambda e, OG=OG: e.tensor_tensor(out=OG[:], in0=OG[:], in1=NWb[:], op=ALU.mult)), [f"og{k}", "NWb"], [f"og{k}"])
                for h in range(H):
                    P.op("dve", (lambda e, A_=A_, S2=S2, h=h: e.bn_stats(out=S2[:, h, :], in_=A_[:, h * DV:(h + 1) * DV])), [f"hA{k}"], [f"st2{k}"])
                    P.op("dve", (lambda e, S2=S2, M2=M2, h=h: e.bn_aggr(out=M2[:, h, 0:2], in_=S2[:, h, :])), [f"st2{k}"], [f"mv2{k}"])
                    P.op("act", (lambda e, M2=M2, h=h: e.activation(out=M2[:, h, 2:3], in_=M2[:, h, 1:2], func=AF.Sqrt, bias=epst[:, 0:1], scale=1.0)), [f"mv2{k}", "epst"], [f"mv2{k}"])
                    P.op("dve", (lambda e, M2=M2, h=h: e.reciprocal(out=M2[:, h, 2:3], in_=M2[:, h, 2:3])), [f"mv2{k}"], [f"mv2{k}"])
                    P.op("dve", (lambda e, M2=M2, h=h: e.scalar_tensor_tensor(out=M2[:, h, 3:4], in0=M2[:, h, 0:1], scalar=-1.0, in1=M2[:, h, 2:3], op0=ALU.mult, op1=ALU.mult)), [f"mv2{k}"], [f"mv2{k}"])
                    P.op("act", (lambda e, A_=A_, M2=M2, h=h: e.activation(out=A_[:, h * DV:(h + 1) * DV], in_=A_[:, h * DV:(h + 1) * DV], func=AF.Identity, bias=M2[:, h, 3:4], scale=M2[:, h, 2:3])), [f"hA{k}", f"mv2{k}"], [f"hA{k}"])
                P.op("pool", (lambda e, A_=A_, OG=OG: e.tensor_tensor(out=A_[:], in0=A_[:], in1=OG[:], op=ALU.mult)), [f"hA{k}", f"og{k}"], [f"hA{k}"])
                for i0 in range(0, MLW // 128, 4):
                    bk = (i0 // 4) % 2
                    for i in range(4):
                        P.op("pe", (lambda e, A_=A_, i=i, i0=i0, bk=bk: e.transpose(out=ps[bk][:, i * 128:(i + 1) * 128], in_=A_[:, (i0 + i) * 128:(i0 + i + 1) * 128], identity=ident)), [f"hA{k}", "cst"], [f"ps{bk}"])
                    P.op("act", (lambda e, MS=MS, i0=i0, bk=bk: e.activation(out=MS[:, i0:i0 + 4, :].rearrange("p c t -> p (c t)"), in_=ps[bk][:], func=AF.Copy)), [f"ps{bk}"], [f"msb{k}"])
                cc0 = HYW // 128
                P.dma("sp", mixv[:, cc0:cc0 + MLW // 128, tl_ * 128:(tl_ + 1) * 128], MS[:], [f"msb{k}"], ["mixT_d"])
            P.flush()

        if "stop_ml" in taps:
            return nc

        latg = [list(range(a, min(a + 4, NOWN))) for a in range(0, NOWN, 4)]
        gemm_tok("wout", mixT_d, DC, w_out, D, latg, gated_epi(lambda gt: gt * 128), gated_setup({gt: 2 for gt in range(NOWN)}))
        tiles = []
        for t_ in range(NOWN):
            tiles.append(dict(src_v=vd_d[t_ * 128:(t_ + 1) * 128, :], src_x=x1_d[(NTC + t_) * 128:(NTC + t_ + 1) * 128, :],
                              dst_x=x2_d[t_ * 128:(t_ + 1) * 128, :], dstT=t_ * 128, r=0, s=2))
        ln_phase("ln2", tiles, cfg.ALPHA, ln_g[1, :], ln_b[1, :])

        nsg = 2 if NOWN > 17 else 1
        per = (NOWN + nsg - 1) // nsg
        for sgi in range(nsg):
            t0 = sgi * per
            t1 = min(NOWN, t0 + per)
            gemm_a(wi[1], t0 * 128, (t1 - t0) * 128)
        gemm_tok("ffn2b", hT_d, FC, wo[1], D, latg, gated_epi(lambda gt: gt * 128), gated_setup({gt: 3 for gt in range(NOWN)}))
        tiles = []
        for t_ in range(NOWN):
            tiles.append(dict(src_v=vd_d[t_ * 128:(t_ + 1) * 128, :], src_x=x2_d[t_ * 128:(t_ + 1) * 128, :],
                              dst_x=out_d[t_ * 128:(t_ + 1) * 128, :]))
        ln_phase("ln3", tiles, cfg.ALPHA, ln_g[2, :], ln_b[2, :])
    return nc


def prep_inputs(cfg, inp, b, consts, flip=False):
    D, DC = cfg.D, cfg.DC
    f32 = lambda a: np.ascontiguousarray(np.asarray(a, dtype=np.float32))
    A = lambda k: np.asarray(inp[k])
    cv = np.stack([A("c")[b], A("c_ctx")], axis=0)
    cvT = cv.reshape(2, DC, 128).transpose(2, 1, 0)
    ctxb, xb = A("ctx")[b], A("x")[b]
    w_in, gate_b = A("w_in")[0], A("ml_gate_b")[0]
    hcw, mcw, w3 = A("hy_conv_w")[0], A("ml_conv_w")[0], A("hy_filt_w3")[0]
    if flip:
        ctxb, xb = ctxb[::-1], xb[::-1]
        hcw, mcw = hcw[::-1], mcw[::-1]
        gperm = np.concatenate([np.arange(8, 16), np.arange(0, 8)])
        w_in = np.concatenate([w_in[:, :cfg.PT - 16], w_in[:, cfg.PT - 16:][:, gperm]], axis=1)
        gate_b = gate_b[gperm]
        w3 = w3.reshape(cfg.FH, 2, 2, cfg.HYW)[:, :, ::-1, :].reshape(cfg.FH, 4 * cfg.HYW)
    m = {
        "xin": f32(np.concatenate([ctxb, xb], axis=0)),
        "cvT": f32(cvT),
        "ada_w": f32(A("ada_w")[0]),
        "ada_bT": f32(A("ada_b")[0].reshape(9 * DC, 128).T),
        "ln_g": f32(A("ln_g")[0]), "ln_b": f32(A("ln_b")[0]),
        "ffn1_wi": f32(A("ffn1_wi")[0]), "ffn1_wo": f32(A("ffn1_wo")[0]),
        "ffn2_wi": f32(A("ffn2_wi")[0]), "ffn2_wo": f32(A("ffn2_wo")[0]),
        "w_in": f32(w_in),
        "hy_conv_w": f32(hcw), "hy_conv_b": f32(A("hy_conv_b")[0][None]),
        "hy_filt_w1": f32(A("hy_filt_w1")[0]), "hy_filt_b1": f32(A("hy_filt_b1")[0][:, None]),
        "hy_filt_f1": f32(A("hy_filt_f1")[0][:, None]),
        "hy_filt_w2": f32(A("hy_filt_w2")[0]), "hy_filt_b2": f32(A("hy_filt_b2")[0][:, None]),
        "hy_filt_f2": f32(A("hy_filt_f2")[0][:, None]),
        "hy_filt_w3": f32(w3), "hy_bias": f32(A("hy_bias")[0]),
        "ml_conv_w": f32(mcw), "ml_conv_b": f32(A("ml_conv_b")[0][None]),
        "ml_gate_b": f32(gate_b[None]), "ml_norm_w": f32(A("ml_norm_w")[0][None]),
        "w_out": f32(A("w_out")[0]),
    }
    m.update(consts)
    return m


def kernel(**inputs):
    cfg = Cfg()
    B = np.asarray(inputs["x"]).shape[0]
    consts = host_consts(cfg)
    half = cfg.NTL // 2
    nc = build(cfg, nown=half)
    shared = {}
    in_maps = []
    for b in range(B):
        for r in range(2):
            m = prep_inputs(cfg, inputs, b, consts, flip=(r == 1))
            for k in list(m.keys()):
                if k in ("xin", "cvT"):
                    continue
                key = (k, r if k in ("w_in", "hy_conv_w", "ml_conv_w", "ml_gate_b", "hy_filt_w3") else 0)
                if key in shared:
                    m[k] = shared[key]
                else:
                    shared[key] = m[k]
            in_maps.append(m)
    res = run_bass_kernel_spmd(nc, in_maps, core_ids=list(range(2 * B)))
    out = np.empty((B, cfg.L, cfg.D), np.float32)
    hl = half * 128
    for b in range(B):
        out[b, :hl] = np.asarray(res.results[2 * b]["out"], dtype=np.float32)
        out[b, hl:] = np.asarray(res.results[2 * b + 1]["out"], dtype=np.float32)[::-1]
    return out
```
